# Optimizing a Trainium2 kernel written in Bass

```python
import math
import jax, jax.numpy as jnp
from jax import lax
import numpy as np

D_MODEL = 2048
BATCH = 2
SEQ = 8192
DEPTH = 1

EPS = 1e-6
ROPE_THETA = 10000.0
Q_BLOCK = 128

MLA_HEADS = 8
MLA_Q_RANK = 768
MLA_KV_RANK = 512
MLA_NOPE = 128
MLA_ROPE = 64
MLA_V = 128
MLA_QK = MLA_NOPE + MLA_ROPE

DIFF_HEADS = 8
DIFF_HD = 64
DIFF_VD = 2 * DIFF_HD

PEER_HEADS = 8
PEER_NKEYS = 128
PEER_EXPERTS = PEER_NKEYS * PEER_NKEYS
PEER_QDIM = 256
PEER_HALF = PEER_QDIM // 2
PEER_TOPK = 16
PEER_CHUNK = 128

OFF_CQ = 0
OFF_CKV = OFF_CQ + MLA_Q_RANK
OFF_KR = OFF_CKV + MLA_KV_RANK
OFF_DQ = OFF_KR + MLA_ROPE
OFF_DK = OFF_DQ + DIFF_HEADS * 2 * DIFF_HD
OFF_DV = OFF_DK + DIFF_HEADS * 2 * DIFF_HD
OFF_GATE = OFF_DV + DIFF_HEADS * DIFF_VD
IN_COLS = OFF_GATE + 2 * D_MODEL

kernel_name = "hybrid_mla_diffattn_peer"


def rmsnorm(x, g):
    xf = x.astype(jnp.float32)
    y = xf * lax.rsqrt(jnp.mean(xf * xf, axis=-1, keepdims=True) + EPS)
    return (y * g.astype(jnp.float32)).astype(x.dtype)


def rope(x, pos):
    d = x.shape[-1]
    inv = ROPE_THETA ** (-jnp.arange(0, d, 2, dtype=jnp.float32) / d)
    ang = pos.astype(jnp.float32)[:, None] * inv[None, :]
    shape = (1, pos.shape[0]) + (1,) * (x.ndim - 3) + (d,)
    cos = jnp.concatenate([jnp.cos(ang), jnp.cos(ang)], -1).reshape(shape)
    sin = jnp.concatenate([jnp.sin(ang), jnp.sin(ang)], -1).reshape(shape)
    xf = x.astype(jnp.float32)
    x1, x2 = xf[..., : d // 2], xf[..., d // 2:]
    rot = jnp.concatenate([-x2, x1], -1)
    return (xf * cos + rot * sin).astype(x.dtype)


def _over_query_blocks(fn, qs):
    b, s = qs[0].shape[:2]
    nb = s // Q_BLOCK
    split = lambda a: a.reshape(b, nb, Q_BLOCK, *a.shape[2:]).swapaxes(0, 1)
    out = lax.map(lambda args: fn(args[0], *args[1:]),
                  (jnp.arange(nb), *[split(a) for a in qs]))
    return out.swapaxes(0, 1).reshape(b, s, *out.shape[3:])


def _causal_probs(q, k, blk, scale):
    qb, s = q.shape[1], k.shape[1]
    sc = jnp.einsum('bqhd,bkhd->bhqk', q, k).astype(jnp.float32) * scale
    qpos = blk * qb + jnp.arange(qb)
    mask = jnp.arange(s)[None, :] <= qpos[:, None]
    sc = jnp.where(mask, sc, -jnp.inf)
    return jax.nn.softmax(sc, axis=-1)


def mla_branch(proj, g_cq, w_uq, g_ckv, w_ukv, pos):
    b, s, _ = proj.shape
    c_q = rmsnorm(proj[..., OFF_CQ:OFF_CKV], g_cq)
    c_kv = rmsnorm(proj[..., OFF_CKV:OFF_KR], g_ckv)
    k_rope = rope(proj[..., OFF_KR:OFF_DQ][:, :, None, :], pos)
    q = (c_q @ w_uq).reshape(b, s, MLA_HEADS, MLA_QK)
    q = jnp.concatenate([q[..., :MLA_NOPE], rope(q[..., MLA_NOPE:], pos)], -1)
    kv = (c_kv @ w_ukv).reshape(b, s, MLA_HEADS, MLA_NOPE + MLA_V)
    k = jnp.concatenate([kv[..., :MLA_NOPE],
                         jnp.broadcast_to(k_rope, (b, s, MLA_HEADS, MLA_ROPE))], -1)
    v = kv[..., MLA_NOPE:]
    scale = MLA_QK ** -0.5

    def attend(blk, q_blk):
        p = _causal_probs(q_blk, k, blk, scale)
        return jnp.einsum('bhqk,bkhd->bqhd', p.astype(v.dtype), v)

    o = _over_query_blocks(attend, [q])
    return o.reshape(b, s, MLA_HEADS * MLA_V)


def diff_branch(proj, lambda_qk, g_subln, lambda_init, pos):
    b, s, _ = proj.shape
    q = rope(proj[..., OFF_DQ:OFF_DK].reshape(b, s, DIFF_HEADS, 2, DIFF_HD), pos)
    k = rope(proj[..., OFF_DK:OFF_DV].reshape(b, s, DIFF_HEADS, 2, DIFF_HD), pos)
    v = proj[..., OFF_DV:OFF_GATE].reshape(b, s, DIFF_HEADS, DIFF_VD)
    q1, q2 = q[..., 0, :], q[..., 1, :]
    k1, k2 = k[..., 0, :], k[..., 1, :]
    lq = lambda_qk.astype(jnp.float32)
    lam = (jnp.exp(jnp.sum(lq[0] * lq[1])) - jnp.exp(jnp.sum(lq[2] * lq[3]))
           + lambda_init)
    scale = DIFF_HD ** -0.5

    def attend(blk, q1b, q2b):
        p = _causal_probs(q1b, k1, blk, scale) - lam * _causal_probs(q2b, k2, blk, scale)
        return jnp.einsum('bhqk,bkhd->bqhd', p.astype(v.dtype), v)

    o = _over_query_blocks(attend, [q1, q2])
    o = rmsnorm(o, g_subln) * (1.0 - lambda_init)
    return o.reshape(b, s, DIFF_HEADS * DIFF_VD)


def peer_ffn(h, w_q, sub_keys, expert_u, expert_v):
    b, s, d = h.shape
    t = b * s
    hf = h.reshape(t, d)
    q = (hf @ w_q).reshape(t, PEER_HEADS, 2, PEER_HALF).astype(jnp.float32)
    sk = sub_keys.astype(jnp.float32)
    s1 = jnp.einsum('thd,nd->thn', q[:, :, 0], sk[0])
    s2 = jnp.einsum('thd,nd->thn', q[:, :, 1], sk[1])
    v1, i1 = lax.top_k(s1, PEER_TOPK)
    v2, i2 = lax.top_k(s2, PEER_TOPK)
    cand = (v1[..., :, None] + v2[..., None, :]).reshape(t, PEER_HEADS, PEER_TOPK * PEER_TOPK)
    cs, ci = lax.top_k(cand, PEER_TOPK)
    row = jnp.take_along_axis(i1, ci // PEER_TOPK, axis=-1)
    col = jnp.take_along_axis(i2, ci % PEER_TOPK, axis=-1)
    experts = row * PEER_NKEYS + col
    gates = jax.nn.softmax(cs, axis=-1)
    nc = t // PEER_CHUNK

    def mix(args):
        hc, ec, gc = args
        a = jax.nn.gelu(jnp.einsum('chkd,cd->chk', expert_u[ec], hc).astype(jnp.float32))
        w = (gc * a).astype(hc.dtype)
        return jnp.einsum('chk,chkd->cd', w, expert_v[ec])

    out = lax.map(mix, (hf.reshape(nc, PEER_CHUNK, d),
                        experts.reshape(nc, PEER_CHUNK, PEER_HEADS, PEER_TOPK),
                        gates.reshape(nc, PEER_CHUNK, PEER_HEADS, PEER_TOPK)))
    return out.reshape(b, s, d)


def setup_inputs(seed: int = 0) -> dict:
    key = jax.random.key(seed)
    ks = jax.random.split(key, 20)
    f32 = jnp.float32
    nrm = lambda k, shape, sc: jax.random.normal(k, shape, f32) * sc
    gain = lambda k, shape: 1.0 + 0.02 * jax.random.normal(k, shape, f32)
    return {
        "x": jax.random.normal(ks[0], (BATCH, SEQ, D_MODEL), f32),
        "w_in": nrm(ks[1], (DEPTH, D_MODEL, IN_COLS), D_MODEL ** -0.5),
        "b_gate": nrm(ks[2], (DEPTH, 2 * D_MODEL), 0.02),
        "g_norm1": gain(ks[3], (DEPTH, D_MODEL)),
        "g_cq": gain(ks[4], (DEPTH, MLA_Q_RANK)),
        "w_uq": nrm(ks[5], (DEPTH, MLA_Q_RANK, MLA_HEADS * MLA_QK), MLA_Q_RANK ** -0.5),
        "g_ckv": gain(ks[6], (DEPTH, MLA_KV_RANK)),
        "w_ukv": nrm(ks[7], (DEPTH, MLA_KV_RANK, MLA_HEADS * (MLA_NOPE + MLA_V)), MLA_KV_RANK ** -0.5),
        "w_o_mla": nrm(ks[8], (DEPTH, MLA_HEADS * MLA_V, D_MODEL), (MLA_HEADS * MLA_V) ** -0.5),
        "lambda_qk": nrm(ks[9], (DEPTH, 4, DIFF_HD), 0.1),
        "g_subln": gain(ks[10], (DEPTH, DIFF_VD)),
        "w_o_diff": nrm(ks[11], (DEPTH, DIFF_HEADS * DIFF_VD, D_MODEL), (DIFF_HEADS * DIFF_VD) ** -0.5),
        "w_out": nrm(ks[12], (DEPTH, D_MODEL, D_MODEL), D_MODEL ** -0.5),
        "g_norm2": gain(ks[13], (DEPTH, D_MODEL)),
        "w_q_peer": nrm(ks[14], (DEPTH, D_MODEL, PEER_HEADS * PEER_QDIM), D_MODEL ** -0.5),
        "sub_keys": nrm(ks[15], (DEPTH, 2, PEER_NKEYS, PEER_HALF), PEER_HALF ** -0.5),
        "expert_u": nrm(ks[16], (DEPTH, PEER_EXPERTS, D_MODEL), D_MODEL ** -0.5),
        "expert_v": nrm(ks[17], (DEPTH, PEER_EXPERTS, D_MODEL), PEER_HEADS ** -0.5),
        "g_final": gain(ks[18], (D_MODEL,)),
    }


def reference(x, w_in, b_gate, g_norm1, g_cq, w_uq, g_ckv, w_ukv, w_o_mla, lambda_qk,
              g_subln, w_o_diff, w_out, g_norm2, w_q_peer, sub_keys, expert_u, expert_v,
              g_final):
    pos = jnp.arange(x.shape[1], dtype=jnp.int32)
    for l in range(DEPTH):
        lambda_init = 0.8 - 0.6 * math.exp(-0.3 * l)
        h = rmsnorm(x, g_norm1[l])
        proj = h @ w_in[l]
        y_a = mla_branch(proj, g_cq[l], w_uq[l], g_ckv[l], w_ukv[l], pos) @ w_o_mla[l]
        y_b = diff_branch(proj, lambda_qk[l], g_subln[l], lambda_init, pos) @ w_o_diff[l]
        gates = jax.nn.sigmoid((proj[..., OFF_GATE:] + b_gate[l]).astype(jnp.float32)).astype(x.dtype)
        merged = gates[..., :D_MODEL] * y_a + gates[..., D_MODEL:] * y_b
        x = x + merged @ w_out[l]
        x = x + peer_ffn(rmsnorm(x, g_norm2[l]), w_q_peer[l], sub_keys[l], expert_u[l], expert_v[l])
    return rmsnorm(x, g_final)
```

```python
import math
from contextlib import ExitStack
import numpy as np
import concourse.bass as bass
import concourse.mybir as mybir
from concourse.bass_utils import run_bass_kernel_spmd

F32 = mybir.dt.float32
BF16 = mybir.dt.bfloat16
ALU = mybir.AluOpType
AF = mybir.ActivationFunctionType
ENGS = ("pe", "act", "dve", "pool", "sp")

D = 2048
SEQ = 8192
T = 512
EPS = 1e-6
NSLOT = 4
NE = 16384
DEBUG = False
_DBG = {}


class _Ins:
    __slots__ = ("eng", "fn", "deps", "dma", "semkey", "val", "signal")

    def __init__(self, eng, fn, dma, semkey):
        self.eng = eng
        self.fn = fn
        self.dma = dma
        self.semkey = semkey
        self.deps = set()
        self.val = None
        self.signal = False


class Prog:
    def __init__(self, nc):
        self.nc = nc
        self.ins = []
        self.last_write = {}
        self.readers = {}
        self.dma_counts = {}
        self.dma_last = {}
        self.eng_last = {}
        self.epoch = set()

    def op(self, eng, fn, reads=(), writes=(), dma=False, semkey=None):
        if dma and semkey is None:
            semkey = ("auto", writes[0])
        i = _Ins(eng, fn, dma, semkey)
        deps = set(self.epoch)
        for b in reads:
            w = self.last_write.get(b)
            if w is not None:
                deps.add(w)
        for b in writes:
            w = self.last_write.get(b)
            if w is not None:
                deps.add(w)
            for r in self.readers.get(b, ()):
                deps.add(r)
        for b in reads:
            self.readers.setdefault(b, []).append(i)
        for b in writes:
            self.last_write[b] = i
            self.readers[b] = []
        if dma:
            c = self.dma_counts.get(semkey, 0) + 16
            self.dma_counts[semkey] = c
            i.val = c
            self.dma_last[semkey] = i
        else:
            self.eng_last[eng] = i
        i.deps = deps
        self.ins.append(i)
        return i

    def barrier(self):
        self.epoch = set(self.eng_last.values()) | set(self.dma_last.values())
        self.last_write = {}
        self.readers = {}

    def pe(self, fn, reads=(), writes=()):
        return self.op("pe", fn, reads, writes)

    def act(self, fn, reads=(), writes=()):
        return self.op("act", fn, reads, writes)

    def dve(self, fn, reads=(), writes=()):
        return self.op("dve", fn, reads, writes)

    def pool(self, fn, reads=(), writes=()):
        return self.op("pool", fn, reads, writes)

    def dma(self, out, in_, reads=(), writes=(), semkey=None):
        return self.op("sp", lambda e: e.dma_start(out=out, in_=in_), reads, writes,
                       dma=True, semkey=semkey)

    def finish(self, final_waits=()):
        nc = self.nc
        for i in self.ins:
            if i.eng == "pe" and not i.dma:
                i.deps = {d for d in i.deps if not (d.eng == "pe" and not d.dma)}
        for i in self.ins:
            for d in i.deps:
                d.signal = True
        for d in final_waits:
            d.signal = True
        cnt = {e: 0 for e in ENGS}
        for i in self.ins:
            if not i.dma and i.signal:
                cnt[i.eng] += 1
                i.val = cnt[i.eng]
        with ExitStack() as es:
            esem = {e: es.enter_context(nc.semaphore("s_" + e)) for e in ENGS}
            dsem = {}
            for k in self.dma_counts:
                dsem[k] = es.enter_context(nc.semaphore("d%d" % len(dsem)))

            def tok(d):
                return (dsem[d.semkey] if d.dma else esem[d.eng]), d.val

            per = {e: [] for e in ENGS}
            for i in self.ins:
                per[i.eng].append(i)
            block = es.enter_context(nc.Block())

            def emit(engname, eh):
                waited = {}
                for i in per[engname]:
                    ws = {}
                    for d in i.deps:
                        s, v = tok(d)
                        key = id(s)
                        if waited.get(key, 0) >= v:
                            continue
                        if key not in ws or ws[key][1] < v:
                            ws[key] = (s, v)
                    for key, (s, v) in ws.items():
                        eh.wait_ge(s, v)
                        waited[key] = v
                    r = i.fn(eh)
                    if i.dma:
                        r.then_inc(dsem[i.semkey], 16)
                    elif i.signal:
                        r.then_inc(esem[i.eng], 1)
                if engname == "sp":
                    for d in final_waits:
                        s, v = tok(d)
                        eh.wait_ge(s, v)

            @block.tensor
            def _(e):
                emit("pe", e)

            @block.scalar
            def _(e):
                emit("act", e)

            @block.vector
            def _(e):
                emit("dve", e)

            @block.gpsimd
            def _(e):
                emit("pool", e)

            @block.sync
            def _(e):
                emit("sp", e)
        return cnt


class Rot:
    def __init__(self, n):
        self.n = n
        self.i = 0

    def nxt(self):
        r = self.i
        self.i = (self.i + 1) % self.n
        return r


C_G1, C_GCQ, C_GCKV, C_BG, C_GSUB, C_G2, C_GF = 0, 16, 22, 26, 58, 59, 75
C_LAM = 91
C_KPOS = 347
C_IOTA = 411
C_IDENT = 539
NC_CONST = 667

NKV = 22
NQ = 22


def build_program():
    nc = bass.Bass("TRN2", target_bir_lowering=False)
    dt_in = lambda n, s: nc.dram_tensor(n, s, F32, kind="ExternalInput").ap()
    xT_seq = dt_in("xT_seq", [D, SEQ])
    xT_own = dt_in("xT_own", [D, 2048])
    cs_seq = dt_in("cs_seq", [2, 128, SEQ])
    cs_own = dt_in("cs_own", [2, 128, 2048])
    qpos_d = dt_in("qpos", [128, 2048])
    consts_d = dt_in("consts", [128, NC_CONST])
    w_kv = dt_in("w_kv", [D, NKV * 128])
    w_dv = dt_in("w_dv", [D, 1024])
    w_q = dt_in("w_q", [D, NQ * 128])
    w_gate = dt_in("w_gate", [D, 4096])
    w_uq2 = dt_in("w_uq2", [768, 2048])
    w_uk = dt_in("w_uk", [512, 1024])
    w_uv = dt_in("w_uv", [512, 1024])
    w_oa = dt_in("w_oa", [1024, D])
    w_ob = dt_in("w_ob", [1024, D])
    w_out = dt_in("w_out", [D, D])
    w_qp = dt_in("w_qp", [D, D])
    skT = dt_in("skT", [2, 128, 128])
    ut = dt_in("ut", [D, NE])
    ev = dt_in("ev", [NE, D])
    outT = nc.dram_tensor("outT", [D, 2048], F32, kind="ExternalOutput").ap()
    skind = "ExternalOutput" if DEBUG else "Internal"
    kTm = nc.dram_tensor("kTm", [8, 128, SEQ], BF16, kind=skind).ap()
    kTr = nc.dram_tensor("kTr", [128, SEQ], BF16, kind=skind).ap()
    kTd = nc.dram_tensor("kTd", [8, 128, SEQ], BF16, kind=skind).ap()
    vM = nc.dram_tensor("vM", [SEQ, 1024], BF16, kind=skind).ap()
    vD = nc.dram_tensor("vD", [SEQ, 1024], BF16, kind=skind).ap()
    taps = {}
    if DEBUG:
        for nm, shp, dtp in (("d_x1", [128, 16, T], F32), ("d_otm", [128, 8, T], BF16), ("d_otd", [128, 8, T], BF16),
                             ("d_qtm", [128, 8, T], BF16), ("d_qtr", [128, 4, T], BF16), ("d_qtd", [128, 8, T], BF16),
                             ("d_h2", [128, 16, T], BF16), ("d_rtt", [128, 3, T], F32), ("d_qp", [128, 16, T], BF16),
                             ("d_xpre", [128, 16, T], F32), ("d_wt", [128, 32, T], BF16)):
            taps[nm] = nc.dram_tensor(nm, shp, dtp, kind="ExternalOutput").ap()

    def tap(nm, tile_ap, ids):
        if DEBUG and nm in taps:
            final_stores.append(P.dma(taps.pop(nm), tile_ap, reads=ids, writes=[("tap", nm)], semkey=("tap", nm)))


    final_stores = []
    es = ExitStack()
    sb = lambda n, s, d: es.enter_context(nc.sbuf_tensor(n, s, d))
    PS = [es.enter_context(nc.psum_tensor("ps%d" % i, [128, 512], F32)) for i in range(8)]
    P = Prog(nc)

    CONST = sb("CONST", [128, NC_CONST], F32)
    ONES = sb("ONES", [128, 128], BF16)
    IDB = sb("IDB", [128, 128], BF16)
    SMALL = sb("SMALL", [128, 16], F32)
    LAMT = sb("LAMT", [128, 128], F32)
    P.dma(CONST[:], consts_d, writes=["CONST"])
    P.pool(lambda e: e.memset(ONES[:], 1.0), writes=["ONES"])
    P.dve(lambda e: e.tensor_copy(out=IDB[:], in_=CONST[:, C_IDENT:C_IDENT + 128]), reads=["CONST"], writes=["IDB"])
    lam_init = 0.8 - 0.6 * math.exp(-0.3 * 0)
    P.dve(lambda e: e.tensor_tensor(out=LAMT[:, 0:64], in0=CONST[:, C_LAM:C_LAM + 64], in1=CONST[:, C_LAM + 64:C_LAM + 128], op=ALU.mult), reads=["CONST"], writes=["LAMT0"])
    P.dve(lambda e: e.tensor_tensor(out=LAMT[:, 64:128], in0=CONST[:, C_LAM + 128:C_LAM + 192], in1=CONST[:, C_LAM + 192:C_LAM + 256], op=ALU.mult), reads=["CONST"], writes=["LAMT1"])
    P.dve(lambda e: e.reduce_sum(out=SMALL[:, 2:3], in_=LAMT[:, 0:64], axis=mybir.AxisListType.X), reads=["LAMT0"], writes=["SM2"])
    P.dve(lambda e: e.reduce_sum(out=SMALL[:, 3:4], in_=LAMT[:, 64:128], axis=mybir.AxisListType.X), reads=["LAMT1"], writes=["SM3"])
    P.act(lambda e: e.activation(out=SMALL[:, 4:6], in_=SMALL[:, 2:4], func=AF.Exp), reads=["SM2", "SM3"], writes=["SM45"])
    P.dve(lambda e: e.tensor_tensor(out=SMALL[:, 6:7], in0=SMALL[:, 5:6], in1=SMALL[:, 4:5], op=ALU.subtract), reads=["SM45"], writes=["SM6"])
    P.dve(lambda e: e.tensor_scalar(out=SMALL[:, 0:1], in0=SMALL[:, 6:7], scalar1=-lam_init, scalar2=None, op0=ALU.add), reads=["SM6"], writes=["NEGLAM"])
    P.dve(lambda e: e.tensor_scalar(out=SMALL[:, 1:2], in0=CONST[:, C_GSUB:C_GSUB + 1], scalar1=1.0 - lam_init, scalar2=None, op0=ALU.mult), reads=["CONST"], writes=["GSUBS"])

    XG = sb("XG", [128, 16, T], BF16)
    RSTD = sb("RSTD", [128, T], F32)
    RSTDT = sb("RSTDT", [128, 8], F32)
    TMPS = sb("TMPS", [128, T], F32)
    WST = [sb("WST%d" % i, [128, 16, 128], F32) for i in range(3)]
    WB = [sb("WB%d" % i, [128, 16, 128], BF16) for i in range(3)]
    wst_rot, wb_rot, ps_rot = Rot(3), Rot(3), Rot(2)

    def stream_w(src, kc, col0, ncols=128):
        si, bi = wst_rot.nxt(), wb_rot.nxt()
        st, wb = WST[si], WB[bi]
        P.dma(st[:, 0:kc, 0:ncols], src[0:kc * 128, col0:col0 + ncols].rearrange("(k p) n -> p k n", p=128),
              writes=[("WST", si)])
        P.pool(lambda e: e.tensor_copy(out=wb[:, 0:kc, 0:ncols], in_=st[:, 0:kc, 0:ncols]), reads=[("WST", si)], writes=[("WB", bi)])
        return wb, ("WB", bi)

    def linear_chunk(src, kc, col0, rhs_fn, rhs_ids, pbank=None):
        wb, wid = stream_w(src, kc, col0)
        bi = ps_rot.nxt() if pbank is None else pbank
        ps = PS[bi]
        for k in range(kc):
            P.pe(lambda e, k=k: e.matmul(ps[:], lhsT=wb[:, k, :], rhs=rhs_fn(k), start=(k == 0), stop=(k == kc - 1)),
                 reads=[wid] + rhs_ids, writes=[("ps", bi)])
        return ps, ("ps", bi)

    def load_x_and_norm(X32, SQ, src, t0, gcol, tokmajor):
        for q in range(4):
            P.dma(X32[:, 4 * q:4 * q + 4, :], src[q * 512:(q + 1) * 512, t0:t0 + T].rearrange("(k p) t -> p k t", p=128),
                  writes=[("X32", q)])
        x32ids = [("X32", q) for q in range(4)]
        for q in range(4):
            P.act(lambda e, q=q: e.activation(out=SQ[:, 4 * q:4 * q + 4, :], in_=X32[:, 4 * q:4 * q + 4, :], func=AF.Square),
                  reads=[("X32", q)], writes=[("SQ", q)])
        sqids = [("SQ", q) for q in range(4)]
        rms_from_sq(SQ, 16, sqids, D, RSTD, "RSTD", RSTDT if tokmajor else None, "RSTDT")
        for k in range(16):
            P.pool(lambda e, k=k: e.tensor_scalar(out=XG[:, k, :], in0=X32[:, k, :], scalar1=CONST[:, gcol + k:gcol + k + 1], scalar2=None, op0=ALU.mult),
                   reads=[("X32", k // 4), "CONST"], writes=[("XG", k)])
        return x32ids, [("XG", k) for k in range(16)]

    def rms_from_sq(sq, kc, sqids, dim, rstd, rid, rstdt, rtid):
        ps = PS[2]
        for k in range(kc):
            P.pe(lambda e, k=k: e.matmul(ps[:], lhsT=ONES[:], rhs=sq[:, k, :], start=(k == 0), stop=(k == kc - 1)),
                 reads=["ONES"] + sqids, writes=[("ps", 2)])
        P.act(lambda e: e.activation(out=rstd[:], in_=ps[:], func=AF.Sqrt, scale=1.0 / dim, bias=EPS), reads=[("ps", 2)], writes=[rid + "s"])
        P.dve(lambda e: e.reciprocal(out=rstd[:], in_=rstd[:]), reads=[rid + "s"], writes=[rid])
        if rstdt is not None:
            ps3 = PS[3]
            for j in range(4):
                for k in range(kc):
                    P.pe(lambda e, k=k, j=j: e.matmul(ps3[:, j:j + 1], lhsT=sq[:, k, j * 128:(j + 1) * 128], rhs=ONES[:, 0:1], start=(k == 0), stop=(k == kc - 1)),
                         reads=["ONES"] + sqids, writes=[("ps", 3)])
            P.act(lambda e: e.activation(out=rstdt[:, 0:4], in_=ps3[:, 0:4], func=AF.Sqrt, scale=1.0 / dim, bias=EPS), reads=[("ps", 3)], writes=[rtid + "s"])
            P.dve(lambda e: e.reciprocal(out=rstdt[:, 0:4], in_=rstdt[:, 0:4]), reads=[rtid + "s"], writes=[rtid])

    def run_phase1():
        with ExitStack() as es1:
            sb1 = lambda n, s, d: es1.enter_context(nc.sbuf_tensor(n, s, d))
            X32 = sb1("X32p1", [128, 16, T], F32)
            SQ = sb1("SQp1", [128, 16, T], BF16)
            WDV = sb1("WDV", [128, 16, 1024], BF16)
            WUK = sb1("WUK", [128, 4, 1024], BF16)
            WUV = sb1("WUV", [128, 4, 1024], BF16)
            CKV32 = sb1("CKV32", [128, 4, T], F32)
            SQ2 = sb1("SQ2", [128, 4, T], BF16)
            RSTD2 = sb1("RSTD2", [128, T], F32)
            CKVN = sb1("CKVN", [128, 4, T], BF16)
            CSK = sb1("CSK", [128, 2, T], F32)
            RA = sb1("RA", [128, T], F32)
            RB = sb1("RB", [128, T], F32)
            KOUT = [sb1("KOUT%d" % i, [128, T], BF16) for i in range(2)]
            VOUT = [sb1("VOUT%d" % i, [128, 1024], BF16) for i in range(2)]
            ko_rot, vo_rot, vps_rot = Rot(2), Rot(2), Rot(2)
            for c8 in range(8):
                wb, wid = stream_w(w_dv, 16, c8 * 128)
                P.pool(lambda e, wb=wb, c8=c8: e.tensor_copy(out=WDV[:, :, c8 * 128:(c8 + 1) * 128], in_=wb[:, :, :]), reads=[wid], writes=[("WDV", c8)])
                wb, wid = stream_w(w_uk, 4, c8 * 128)
                P.pool(lambda e, wb=wb, c8=c8: e.tensor_copy(out=WUK[:, :, c8 * 128:(c8 + 1) * 128], in_=wb[:, 0:4, :]), reads=[wid], writes=[("WUK", c8)])
                wb, wid = stream_w(w_uv, 4, c8 * 128)
                P.pool(lambda e, wb=wb, c8=c8: e.tensor_copy(out=WUV[:, :, c8 * 128:(c8 + 1) * 128], in_=wb[:, 0:4, :]), reads=[wid], writes=[("WUV", c8)])
            wdv_ids = [("WDV", c) for c in range(8)]
            wuk_ids = [("WUK", c) for c in range(8)]
            wuv_ids = [("WUV", c) for c in range(8)]

            def rope_store(psA_fn, psB_fn, dst):
                psA, idA = psA_fn()
                P.dve(lambda e: e.tensor_tensor(out=RA[:], in0=psA[:], in1=RSTD[:], op=ALU.mult), reads=[idA, "RSTD"], writes=["RA"])
                psB, idB = psB_fn()
                P.dve(lambda e: e.tensor_tensor(out=RB[:], in0=psB[:], in1=RSTD[:], op=ALU.mult), reads=[idB, "RSTD"], writes=["RB"])
                P.pool(lambda e: e.tensor_tensor(out=RA[:], in0=RA[:], in1=CSK[:, 0, :], op=ALU.mult), reads=["RA", "CSK"], writes=["RA"])
                P.pool(lambda e: e.tensor_tensor(out=RB[:], in0=RB[:], in1=CSK[:, 1, :], op=ALU.mult), reads=["RB", "CSK"], writes=["RB"])
                ki = ko_rot.nxt()
                P.pool(lambda e: e.tensor_tensor(out=KOUT[ki][:], in0=RA[:], in1=RB[:], op=ALU.add), reads=["RA", "RB"], writes=[("KOUT", ki)])
                P.dma(dst, KOUT[ki][:], reads=[("KOUT", ki)], writes=[("kscr", ki)], semkey=("kst", ki))

            for it in range(SEQ // T):
                t0 = it * T
                x32ids, xgids = load_x_and_norm(X32, SQ, xT_seq, t0, C_G1, True)
                P.dma(CSK[:], cs_seq[:, :, t0:t0 + T].rearrange("c p t -> p c t"), writes=["CSK"])
                rhs_x = lambda k: XG[:, k, :]
                for n in range(4):
                    ps, pid = linear_chunk(w_kv, 16, n * 128, rhs_x, xgids)
                    P.dve(lambda e, ps=ps, n=n: e.tensor_tensor(out=CKV32[:, n, :], in0=ps[:], in1=RSTD[:], op=ALU.mult), reads=[pid, "RSTD"], writes=[("CKV32", n)])
                    P.act(lambda e, n=n: e.activation(out=SQ2[:, n, :], in_=CKV32[:, n, :], func=AF.Square), reads=[("CKV32", n)], writes=[("SQ2", n)])
                rope_store(lambda: linear_chunk(w_kv, 16, 4 * 128, rhs_x, xgids), lambda: linear_chunk(w_kv, 16, 5 * 128, rhs_x, xgids), kTr[:, t0:t0 + T])
                for h in range(8):
                    rope_store(lambda h=h: linear_chunk(w_kv, 16, (6 + 2 * h) * 128, rhs_x, xgids),
                               lambda h=h: linear_chunk(w_kv, 16, (7 + 2 * h) * 128, rhs_x, xgids), kTd[h, :, t0:t0 + T])
                sq2ids = [("SQ2", n) for n in range(4)]
                rms_from_sq(SQ2, 4, sq2ids, 512, RSTD2, "RSTD2", None, None)
                for n in range(4):
                    P.dve(lambda e, n=n: e.scalar_tensor_tensor(out=CKVN[:, n, :], in0=CKV32[:, n, :], scalar=CONST[:, C_GCKV + n:C_GCKV + n + 1], in1=RSTD2[:], op0=ALU.mult, op1=ALU.mult),
                          reads=[("CKV32", n), "RSTD2", "CONST"], writes=[("CKVN", n)])
                ckvn_ids = [("CKVN", n) for n in range(4)]
                for h in range(8):
                    bi = ps_rot.nxt()
                    ps = PS[bi]
                    for j in range(4):
                        P.pe(lambda e, ps=ps, j=j, h=h: e.matmul(ps[:], lhsT=WUK[:, j, h * 128:(h + 1) * 128], rhs=CKVN[:, j, :], start=(j == 0), stop=(j == 3)),
                             reads=wuk_ids + ckvn_ids, writes=[("ps", bi)])
                    ki = ko_rot.nxt()
                    P.act(lambda e, ps=ps, ki=ki: e.activation(out=KOUT[ki][:], in_=ps[:], func=AF.Copy), reads=[("ps", bi)], writes=[("KOUT", ki)])
                    P.dma(kTm[h, :, t0:t0 + T], KOUT[ki][:], reads=[("KOUT", ki)], writes=[("kscr", ki)], semkey=("kst", ki))
                for j4 in range(4):
                    vi = vo_rot.nxt()
                    for half in range(2):
                        bi = 4 + vps_rot.nxt()
                        ps = PS[bi]
                        for j in range(4):
                            P.pe(lambda e, ps=ps, j=j, j4=j4, half=half: e.matmul(ps[:], lhsT=CKVN[:, j, j4 * 128:(j4 + 1) * 128], rhs=WUV[:, j, half * 512:(half + 1) * 512], start=(j == 0), stop=(j == 3)),
                                 reads=wuv_ids + ckvn_ids, writes=[("ps", bi)])
                        P.act(lambda e, ps=ps, vi=vi, half=half: e.activation(out=VOUT[vi][:, half * 512:(half + 1) * 512], in_=ps[:], func=AF.Copy), reads=[("ps", bi)], writes=[("VOUT", vi, half)])
                    P.dma(vM[t0 + j4 * 128:t0 + (j4 + 1) * 128, :], VOUT[vi][:], reads=[("VOUT", vi, 0), ("VOUT", vi, 1)], writes=[("vscr", vi)], semkey=("vst", vi))
                    vi = vo_rot.nxt()
                    for half in range(2):
                        bi = 4 + vps_rot.nxt()
                        ps = PS[bi]
                        for k in range(16):
                            P.pe(lambda e, ps=ps, k=k, j4=j4, half=half: e.matmul(ps[:], lhsT=XG[:, k, j4 * 128:(j4 + 1) * 128], rhs=WDV[:, k, half * 512:(half + 1) * 512], start=(k == 0), stop=(k == 15)),
                                 reads=wdv_ids + xgids, writes=[("ps", bi)])
                        P.act(lambda e, ps=ps, vi=vi, half=half, j4=j4: e.activation(out=VOUT[vi][:, half * 512:(half + 1) * 512], in_=ps[:], func=AF.Copy, scale=RSTDT[:, j4:j4 + 1]),
                              reads=[("ps", bi), "RSTDT"], writes=[("VOUT", vi, half)])
                    P.dma(vD[t0 + j4 * 128:t0 + (j4 + 1) * 128, :], VOUT[vi][:], reads=[("VOUT", vi, 0), ("VOUT", vi, 1)], writes=[("vscr", vi)], semkey=("vst", vi))
            P.barrier()


    run_phase1()

    def run_slot(s):
        t0 = s * T
        nkt = 16 * (s + 1)
        with ExitStack() as esS:
            sbS = lambda n, sh, d: esS.enter_context(nc.sbuf_tensor("%s_%d" % (n, s), sh, d))
            X32 = sbS("X32", [128, 16, T], F32)
            SQ = sbS("SQ", [128, 16, T], BF16)
            with ExitStack() as esO:
                sbO = lambda n, sh, d: esO.enter_context(nc.sbuf_tensor("%s_%d" % (n, s), sh, d))
                OTM = sbO("OTM", [128, 8, T], BF16)
                OTD = sbO("OTD", [128, 8, T], BF16)
                with ExitStack() as esQ:
                    sbQ = lambda n, sh, d: esQ.enter_context(nc.sbuf_tensor("%s_%d" % (n, s), sh, d))
                    QTM = sbQ("QTM", [128, 8, T], BF16)
                    QTR = sbQ("QTR", [128, 4, T], BF16)
                    QTD = sbQ("QTD", [128, 8, T], BF16)
                    with ExitStack() as esA:
                        sbA = lambda n, sh, d: esA.enter_context(nc.sbuf_tensor("%s_%d" % (n, s), sh, d))
                        CQ32 = sbA("CQ32", [128, 6, T], F32)
                        SQ3 = sbA("SQ3", [128, 6, T], BF16)
                        CQN = sbA("CQN", [128, 6, T], BF16)
                        CSQ = sbA("CSQ", [128, 2, T], F32)
                        RA = sbA("RA2", [128, T], F32)
                        RB = sbA("RB2", [128, T], F32)
                        x32ids, xgids = load_x_and_norm(X32, SQ, xT_own, t0, C_G1, False)
                        P.dma(CSQ[:], cs_own[:, :, t0:t0 + T].rearrange("c p t -> p c t"), writes=["CSQ"])
                        rhs_x = lambda k: XG[:, k, :]

                        def rope_q(psA_fn, psB_fn, dst, dst_id, scaled):
                            psA, idA = psA_fn()
                            if scaled:
                                P.dve(lambda e: e.tensor_tensor(out=RA[:], in0=psA[:], in1=RSTD[:], op=ALU.mult), reads=[idA, "RSTD"], writes=["RA"])
                            else:
                                P.act(lambda e: e.activation(out=RA[:], in_=psA[:], func=AF.Copy), reads=[idA], writes=["RA"])
                            psB, idB = psB_fn()
                            if scaled:
                                P.dve(lambda e: e.tensor_tensor(out=RB[:], in0=psB[:], in1=RSTD[:], op=ALU.mult), reads=[idB, "RSTD"], writes=["RB"])
                            else:
                                P.act(lambda e: e.activation(out=RB[:], in_=psB[:], func=AF.Copy), reads=[idB], writes=["RB"])
                            P.pool(lambda e: e.tensor_tensor(out=RA[:], in0=RA[:], in1=CSQ[:, 0, :], op=ALU.mult), reads=["RA", "CSQ"], writes=["RA"])
                            P.pool(lambda e: e.tensor_tensor(out=RB[:], in0=RB[:], in1=CSQ[:, 1, :], op=ALU.mult), reads=["RB", "CSQ"], writes=["RB"])
                            P.pool(lambda e: e.tensor_tensor(out=dst, in0=RA[:], in1=RB[:], op=ALU.add), reads=["RA", "RB"], writes=[dst_id])

                        for n in range(6):
                            ps, pid = linear_chunk(w_q, 16, n * 128, rhs_x, xgids)
                            P.dve(lambda e, ps=ps, n=n: e.tensor_tensor(out=CQ32[:, n, :], in0=ps[:], in1=RSTD[:], op=ALU.mult), reads=[pid, "RSTD"], writes=[("CQ32", n)])
                            P.act(lambda e, n=n: e.activation(out=SQ3[:, n, :], in_=CQ32[:, n, :], func=AF.Square), reads=[("CQ32", n)], writes=[("SQ3", n)])
                        for h in range(8):
                            rope_q(lambda h=h: linear_chunk(w_q, 16, (6 + 2 * h) * 128, rhs_x, xgids),
                                   lambda h=h: linear_chunk(w_q, 16, (7 + 2 * h) * 128, rhs_x, xgids), QTD[:, h, :], ("QTD", h), True)
                        sq3ids = [("SQ3", n) for n in range(6)]
                        rms_from_sq(SQ3, 6, sq3ids, 768, TMPS, "TMPS", None, None)
                        for n in range(6):
                            P.dve(lambda e, n=n: e.scalar_tensor_tensor(out=CQN[:, n, :], in0=CQ32[:, n, :], scalar=CONST[:, C_GCQ + n:C_GCQ + n + 1], in1=TMPS[:], op0=ALU.mult, op1=ALU.mult),
                                  reads=[("CQ32", n), "TMPS", "CONST"], writes=[("CQN", n)])
                        cqn_ids = [("CQN", n) for n in range(6)]
                        rhs_c = lambda k: CQN[:, k, :]
                        for h in range(8):
                            ps, pid = linear_chunk(w_uq2, 6, h * 128, rhs_c, cqn_ids)
                            P.act(lambda e, ps=ps, h=h: e.activation(out=QTM[:, h, :], in_=ps[:], func=AF.Copy), reads=[pid], writes=[("QTM", h)])
                        for j in range(4):
                            rope_q(lambda j=j: linear_chunk(w_uq2, 6, (8 + j) * 128, rhs_c, cqn_ids),
                                   lambda j=j: linear_chunk(w_uq2, 6, (12 + j) * 128, rhs_c, cqn_ids), QTR[:, j, :], ("QTR", j), False)
                        tap("d_qtm", QTM[:], [("QTM", h) for h in range(8)])
                        tap("d_qtr", QTR[:], [("QTR", h) for h in range(4)])
                        tap("d_qtd", QTD[:], [("QTD", h) for h in range(8)])
                        P.barrier()
                    with ExitStack() as esB:
                        sbB = lambda n, sh, d: esB.enter_context(nc.sbuf_tensor("%s_%d" % (n, s), sh, d))
                        QPOS = sbB("QPOS", [128, T], F32)
                        KC = [sbB("KC%d" % i, [128, 2048], BF16) for i in range(3)]
                        KRC = [sbB("KRC%d" % i, [128, 2048], BF16) for i in range(3)]
                        VC = [sbB("VC%d" % i, [128, 16, 128], BF16) for i in range(3)]
                        PT = [sbB("PT%d" % i, [128, T], BF16) for i in range(3)]
                        A1 = sbB("A1", [128, T], F32)
                        A2 = sbB("A2", [128, T], F32)
                        RL = sbB("RL", [128, T], F32)
                        SQD = sbB("SQD", [128, 1, T], BF16)
                        kc_rot, pt_rot = Rot(3), Rot(3)
                        P.dma(QPOS[:], qpos_d[:, t0:t0 + T], writes=["QPOS"])

                        def attend(ksrc, vsrc, h, with_rope, streams):
                            for kt in range(nkt):
                                c16, kk = kt // 16, kt % 16
                                if kk == 0:
                                    ci = kc_rot.nxt()
                                    ks = slice(c16 * 2048, (c16 + 1) * 2048)
                                    P.dma(KC[ci][:], ksrc[h, :, ks], writes=[("KC", ci)])
                                    P.dma(VC[ci][:], vsrc[ks, h * 128:(h + 1) * 128].rearrange("(kt p) d -> p kt d", p=128), writes=[("VC", ci)])
                                    kids = [("KC", ci)]
                                    if with_rope:
                                        P.dma(KRC[ci][:], kTr[:, ks], writes=[("KRC", ci)])
                                        kids.append(("KRC", ci))
                                for (parts_fn, q_ids, scale, o_bank, l_bank) in streams:
                                    bi = ps_rot.nxt()
                                    ps = PS[bi]
                                    parts = parts_fn(ci, kk)
                                    for pi, (lh, rh) in enumerate(parts):
                                        P.pe(lambda e, ps=ps, lh=lh, rh=rh, pi=pi, n=len(parts): e.matmul(ps[:], lhsT=lh, rhs=rh, start=(pi == 0), stop=(pi == n - 1)),
                                             reads=kids + q_ids, writes=[("ps", bi)])
                                    pti = pt_rot.nxt()
                                    pt = PT[pti]
                                    P.act(lambda e, ps=ps, pt=pt, scale=scale: e.activation(out=pt[:], in_=ps[:], func=AF.Exp, scale=scale), reads=[("ps", bi)], writes=[("PT", pti)])
                                    if kt >= 16 * s:
                                        P.dve(lambda e, pt=pt, kt=kt: e.scalar_tensor_tensor(out=pt[:], in0=QPOS[:], scalar=CONST[:, C_KPOS + kt:C_KPOS + kt + 1], in1=pt[:], op0=ALU.is_ge, op1=ALU.mult),
                                              reads=[("PT", pti), "QPOS", "CONST"], writes=[("PT", pti)])
                                    P.pe(lambda e, pt=pt, kt=kt, ci=ci, kk=kk, o_bank=o_bank: e.matmul(PS[o_bank][:], lhsT=VC[ci][:, kk, :], rhs=pt[:], start=(kt == 0), stop=(kt == nkt - 1)),
                                         reads=[("PT", pti), ("VC", ci)], writes=[("ps", o_bank)])
                                    P.pe(lambda e, pt=pt, kt=kt, l_bank=l_bank: e.matmul(PS[l_bank][:], lhsT=ONES[:], rhs=pt[:], start=(kt == 0), stop=(kt == nkt - 1)),
                                         reads=[("PT", pti), "ONES"], writes=[("ps", l_bank)])

                        for h in range(8):
                            hb = 64 * (h % 2)
                            attend(kTm, vM, h, True,
                                   [(lambda ci, kk, h=h, hb=hb: [(KC[ci][:, kk * 128:(kk + 1) * 128], QTM[:, h, :]),
                                                                 (KRC[ci][hb:hb + 64, kk * 128:(kk + 1) * 128], QTR[hb:hb + 64, h // 2, :])],
                                     [("QTM", h), ("QTR", h // 2)], 192.0 ** -0.5, 2, 3)])
                            P.dve(lambda e: e.reciprocal(out=RL[:], in_=PS[3][:]), reads=[("ps", 3)], writes=["RL"])
                            P.dve(lambda e, h=h: e.tensor_tensor(out=OTM[:, h, :], in0=PS[2][:], in1=RL[:], op=ALU.mult), reads=[("ps", 2), "RL"], writes=[("OTM", h)])
                        for h in range(8):
                            attend(kTd, vD, h, False,
                                   [(lambda ci, kk, h=h, m=m: [(KC[ci][64 * m:64 * m + 64, kk * 128:(kk + 1) * 128], QTD[64 * m:64 * m + 64, h, :])],
                                     [("QTD", h)], 64.0 ** -0.5, 2 + 2 * m, 3 + 2 * m) for m in range(2)])
                            P.dve(lambda e: e.reciprocal(out=RL[:], in_=PS[3][:]), reads=[("ps", 3)], writes=["RL"])
                            P.dve(lambda e: e.tensor_tensor(out=A1[:], in0=PS[2][:], in1=RL[:], op=ALU.mult), reads=[("ps", 2), "RL"], writes=["A1"])
                            P.dve(lambda e: e.reciprocal(out=RL[:], in_=PS[5][:]), reads=[("ps", 5), "A1"], writes=["RL"])
                            P.dve(lambda e: e.scalar_tensor_tensor(out=A2[:], in0=PS[4][:], scalar=SMALL[:, 0:1], in1=RL[:], op0=ALU.mult, op1=ALU.mult), reads=[("ps", 4), "RL", "NEGLAM"], writes=["A2"])
                            P.pool(lambda e: e.tensor_tensor(out=A1[:], in0=A1[:], in1=A2[:], op=ALU.add), reads=["A1", "A2"], writes=["A1"])
                            P.act(lambda e: e.activation(out=SQD[:, 0, :], in_=A1[:], func=AF.Square), reads=["A1"], writes=["SQD"])
                            rms_from_sq(SQD, 1, ["SQD"], 128, A2, "A2", None, None)
                            P.dve(lambda e, h=h: e.scalar_tensor_tensor(out=OTD[:, h, :], in0=A1[:], scalar=SMALL[:, 1:2], in1=A2[:], op0=ALU.mult, op1=ALU.mult),
                                  reads=["A1", "A2", "GSUBS"], writes=[("OTD", h)])
                        tap("d_otm", OTM[:], [("OTM", h) for h in range(8)])
                        tap("d_otd", OTD[:], [("OTD", h) for h in range(8)])
                        P.barrier()
                with ExitStack() as esC:
                    sbC = lambda n, sh, d: esC.enter_context(nc.sbuf_tensor("%s_%d" % (n, s), sh, d))
                    MG = sbC("MG", [128, 16, T], BF16)
                    M1 = sbC("M1", [128, T], F32)
                    M2 = sbC("M2", [128, T], F32)
                    GT_ = sbC("GT_", [128, T], F32)
                    xgids = [("XG", k) for k in range(16)]
                    rhs_x = lambda k: XG[:, k, :]
                    otm_ids = [("OTM", h) for h in range(8)]
                    otd_ids = [("OTD", h) for h in range(8)]
                    for n in range(16):
                        for br, (wo, OT, oids, mdst, mid) in enumerate(((w_oa, OTM, otm_ids, M1, "M1"), (w_ob, OTD, otd_ids, M2, "M2"))):
                            psg, pgid = linear_chunk(w_gate, 16, (br * 16 + n) * 128, rhs_x, xgids)
                            P.dve(lambda e, psg=psg: e.tensor_tensor(out=GT_[:], in0=psg[:], in1=RSTD[:], op=ALU.mult), reads=[pgid, "RSTD"], writes=["GT_"])
                            P.act(lambda e, br=br, n=n: e.activation(out=GT_[:], in_=GT_[:], func=AF.Sigmoid, bias=CONST[:, C_BG + br * 16 + n:C_BG + br * 16 + n + 1]), reads=["GT_", "CONST"], writes=["GT_"])
                            psy, pyid = linear_chunk(wo, 8, n * 128, lambda k, OT=OT: OT[:, k, :], oids)
                            P.dve(lambda e, psy=psy, mdst=mdst: e.tensor_tensor(out=mdst[:], in0=psy[:], in1=GT_[:], op=ALU.mult), reads=[pyid, "GT_"], writes=[mid])
                        P.pool(lambda e, n=n: e.tensor_tensor(out=MG[:, n, :], in0=M1[:], in1=M2[:], op=ALU.add), reads=["M1", "M2"], writes=[("MG", n)])
                    mg_ids = [("MG", n) for n in range(16)]
                    for n in range(16):
                        ps, pid = linear_chunk(w_out, 16, n * 128, lambda k: MG[:, k, :], mg_ids)
                        P.dve(lambda e, ps=ps, n=n: e.tensor_tensor(out=X32[:, n, :], in0=X32[:, n, :], in1=ps[:], op=ALU.add), reads=[pid, ("X32", n // 4)], writes=[("X32", n // 4)])
                    tap("d_x1", X32[:], [("X32", q) for q in range(4)])
                    P.barrier()

            with ExitStack() as es3:
                sb3 = lambda n, sh, d: es3.enter_context(nc.sbuf_tensor("%s_%d" % (n, s), sh, d))
                QP = SQ
                SKB = sb3("SKB", [128, 2, 128], BF16)
                S32 = sb3("S32", [128, 16, 128], F32)
                TK = sb3("TK", [128, 2, 128], F32)
                V12 = sb3("V12", [128, 2, 16], F32)
                I1 = sb3("I1", [128, 2, 16], mybir.dt.uint32)
                I1F = sb3("I1F", [128, 2, 16], F32)
                CAND = sb3("CAND", [128, 256], F32)
                CAND2 = sb3("CAND2", [128, 256], F32)
                C16 = sb3("C16", [128, 16], F32)
                CI = sb3("CI", [128, 16], mybir.dt.uint32)
                CIH = sb3("CIH", [128, 16], mybir.dt.uint32)
                CIL = sb3("CIL", [128, 16], mybir.dt.uint32)
                CIHF = sb3("CIHF", [128, 16], F32)
                CILF = sb3("CILF", [128, 16], F32)
                OH = sb3("OH", [128, 16, 16], F32)
                SM = sb3("SM", [128, 8], F32)
                EJ = sb3("EJ", [128, 16], F32)
                ROWC = sb3("ROWC", [128, 3, 128], BF16)
                RTT = sb3("RTT", [128, 3, T], F32)
                WT = sb3("WT", [128, 32, T], BF16)
                G8 = [sb3("G8_%d" % i, [128, 8, 128], BF16) for i in range(2)]
                R8 = [sb3("R8_%d" % i, [128, 8, 32], BF16) for i in range(2)]
                VB = [sb3("VB%d" % i, [128, D], BF16) for i in range(6)]
                GA = [sb3("GA%d" % i, [128, T], BF16) for i in range(2)]
                PP = [sb3("PP%d" % i, [128, T], BF16) for i in range(5)]
                g8_rot, vb_rot, ga_rot, pp_rot, ops_rot = Rot(2), Rot(6), Rot(2), Rot(5), Rot(2)

                for q in range(4):
                    P.act(lambda e, q=q: e.activation(out=SQ[:, 4 * q:4 * q + 4, :], in_=X32[:, 4 * q:4 * q + 4, :], func=AF.Square), reads=[("X32", q)], writes=[("SQ", q)])
                rms_from_sq(SQ, 16, [("SQ", q) for q in range(4)], D, RSTD, "RSTD", None, None)
                for k in range(16):
                    P.dve(lambda e, k=k: e.scalar_tensor_tensor(out=XG[:, k, :], in0=X32[:, k, :], scalar=CONST[:, C_G2 + k:C_G2 + k + 1], in1=RSTD[:], op0=ALU.mult, op1=ALU.mult),
                          reads=[("X32", k // 4), "RSTD", "CONST"], writes=[("XG", k)])
                h2ids = [("XG", k) for k in range(16)]
                rhs_h = lambda k: XG[:, k, :]
                si0 = wst_rot.nxt()
                P.dma(WST[si0][:, 0:2, :], skT.rearrange("c p n -> p c n"), writes=[("WST", si0)])
                P.pool(lambda e: e.tensor_copy(out=SKB[:], in_=WST[si0][:, 0:2, :]), reads=[("WST", si0)], writes=["SKB"])
                for n in range(16):
                    ps, pid = linear_chunk(w_qp, 16, n * 128, rhs_h, h2ids)
                    P.act(lambda e, ps=ps, n=n: e.activation(out=QP[:, n, :], in_=ps[:], func=AF.Copy), reads=[pid], writes=[("SQ", n // 4)])

                for j4 in range(4):
                    tsl = slice(j4 * 128, (j4 + 1) * 128)
                    for g in range(4):
                        bi = 3 + (g % 2)
                        for jj in range(4):
                            n = g * 4 + jj
                            P.pe(lambda e, bi=bi, jj=jj, n=n, tsl=tsl: e.matmul(PS[bi][:, jj * 128:(jj + 1) * 128], lhsT=QP[:, n, tsl], rhs=SKB[:, n % 2, :], start=True, stop=True),
                                 reads=[("SQ", n // 4), "SKB"], writes=[("ps", bi)])
                        P.act(lambda e, bi=bi, g=g: e.activation(out=S32[:, 4 * g:4 * g + 4, :], in_=PS[bi][:].rearrange("p (a b) -> p a b", a=4), func=AF.Copy), reads=[("ps", bi)], writes=[("S32", g)])
                    for h in range(8):
                        sid = ("S32", h // 2)
                        for half in range(2):
                            sc = S32[:, 2 * h + half, :]
                            tk = TK[:, half, :]
                            P.dve(lambda e, sc=sc, half=half: e.max(out=V12[:, half, 0:8], in_=sc), reads=[sid], writes=["V12"])
                            P.dve(lambda e, sc=sc, half=half: e.max_index(out=I1[:, half, 0:8], in_max=V12[:, half, 0:8], in_values=sc), reads=[sid, "V12"], writes=["I1"])
                            P.dve(lambda e, sc=sc, tk=tk, half=half: e.match_replace(out=tk, in_to_replace=V12[:, half, 0:8], in_values=sc, imm_value=-1e30), reads=[sid, "V12"], writes=["TK"])
                            P.dve(lambda e, tk=tk, half=half: e.max(out=V12[:, half, 8:16], in_=tk), reads=["TK"], writes=["V12"])
                            P.dve(lambda e, tk=tk, half=half: e.max_index(out=I1[:, half, 8:16], in_max=V12[:, half, 8:16], in_values=tk), reads=["TK", "V12"], writes=["I1"])
                        P.dve(lambda e: e.tensor_copy(out=I1F[:], in_=I1[:]), reads=["I1"], writes=["I1F"])
                        P.dve(lambda e: e.tensor_tensor(out=CAND[:].rearrange("p (a b) -> p a b", a=16), in0=V12[:, 0, :].unsqueeze(2).broadcast_to([128, 16, 16]),
                                                        in1=V12[:, 1, :].unsqueeze(1).broadcast_to([128, 16, 16]), op=ALU.add), reads=["V12"], writes=["CAND"])
                        P.dve(lambda e: e.max(out=C16[:, 0:8], in_=CAND[:]), reads=["CAND"], writes=["C16"])
                        P.dve(lambda e: e.max_index(out=CI[:, 0:8], in_max=C16[:, 0:8], in_values=CAND[:]), reads=["CAND", "C16"], writes=["CI"])
                        P.dve(lambda e: e.match_replace(out=CAND2[:], in_to_replace=C16[:, 0:8], in_values=CAND[:], imm_value=-1e30), reads=["CAND", "C16"], writes=["CAND2"])
                        P.dve(lambda e: e.max(out=C16[:, 8:16], in_=CAND2[:]), reads=["CAND2"], writes=["C16"])
                        P.dve(lambda e: e.max_index(out=CI[:, 8:16], in_max=C16[:, 8:16], in_values=CAND2[:]), reads=["CAND2", "C16"], writes=["CI"])
                        P.dve(lambda e: e.tensor_scalar(out=SM[:, 0:1], in0=C16[:, 0:1], scalar1=-1.0, scalar2=None, op0=ALU.mult), reads=["C16"], writes=["SM0"])
                        P.act(lambda e: e.activation(out=EJ[:], in_=C16[:], func=AF.Exp, bias=SM[:, 0:1], accum_out=SM[:, 1:2]), reads=["C16", "SM0"], writes=["EJ", "SM1"])
                        P.dve(lambda e: e.reciprocal(out=SM[:, 2:3], in_=SM[:, 1:2]), reads=["SM1"], writes=["SM2"])
                        P.dve(lambda e, h=h: e.tensor_scalar(out=ROWC[:, 2, h * 16:(h + 1) * 16], in0=EJ[:], scalar1=SM[:, 2:3], scalar2=None, op0=ALU.mult), reads=["EJ", "SM2"], writes=[("ROWC", 2, h)])
                        P.dve(lambda e: e.tensor_single_scalar(out=CIH[:], in_=CI[:], scalar=4, op=ALU.logical_shift_right), reads=["CI"], writes=["CIH"])
                        P.dve(lambda e: e.tensor_single_scalar(out=CIL[:], in_=CI[:], scalar=15, op=ALU.bitwise_and), reads=["CI"], writes=["CIL"])
                        P.dve(lambda e: e.tensor_copy(out=CIHF[:], in_=CIH[:]), reads=["CIH"], writes=["CIHF"])
                        P.dve(lambda e: e.tensor_copy(out=CILF[:], in_=CIL[:]), reads=["CIL"], writes=["CILF"])
                        for which, (cf, cid) in enumerate(((CIHF, "CIHF"), (CILF, "CILF"))):
                            P.dve(lambda e, cf=cf: e.tensor_tensor(out=OH[:], in0=CONST[:, C_IOTA:C_IOTA + 16].unsqueeze(1).broadcast_to([128, 16, 16]),
                                                                   in1=cf[:].unsqueeze(2).broadcast_to([128, 16, 16]), op=ALU.is_equal), reads=["CONST", cid, "EJ"], writes=["OH"])
                            P.dve(lambda e, which=which: e.tensor_tensor(out=OH[:], in0=OH[:], in1=I1F[:, which, :].unsqueeze(1).broadcast_to([128, 16, 16]), op=ALU.mult), reads=["OH", "I1F"], writes=["OH"])
                            P.dve(lambda e: e.reduce_sum(out=EJ[:], in_=OH[:], axis=mybir.AxisListType.X), reads=["OH"], writes=["EJ"])
                            P.dve(lambda e, which=which, h=h: e.tensor_copy(out=ROWC[:, which, h * 16:(h + 1) * 16], in_=EJ[:]), reads=["EJ"], writes=[("ROWC", which, h)])
                    rc_ids = [("ROWC", w, h) for w in range(3) for h in range(8)]
                    for w in range(3):
                        P.pe(lambda e, w=w: e.matmul(PS[5][:, w * 128:(w + 1) * 128], lhsT=ROWC[:, w, :], rhs=IDB[:], start=True, stop=True), reads=rc_ids + ["IDB"], writes=[("ps", 5)])
                    P.act(lambda e, tsl=tsl: e.activation(out=RTT[:, :, tsl], in_=PS[5][:, 0:384].rearrange("p (a b) -> p a b", a=3), func=AF.Copy), reads=[("ps", 5)], writes=[("RTT", j4)])
                rtt_ids = [("RTT", j) for j in range(4)]
                tap("d_rtt", RTT[:], rtt_ids)
                tap("d_h2", XG[:], h2ids)
                tap("d_qp", QP[:], [("SQ", q) for q in range(4)])

                for qr in range(4):
                    for b8 in range(T // 8):
                        gi = g8_rot.nxt()
                        ts8 = slice(b8 * 8, b8 * 8 + 8)
                        P.dve(lambda e, gi=gi, ts8=ts8: e.tensor_tensor(out=G8[gi][:], in0=CONST[:, C_IOTA:C_IOTA + 128].unsqueeze(1).broadcast_to([128, 8, 128]),
                                                                       in1=RTT[:, 1, ts8].unsqueeze(2).broadcast_to([128, 8, 128]), op=ALU.is_equal), reads=rtt_ids + ["CONST"], writes=[("G8", gi)])
                        P.pool(lambda e, gi=gi, ts8=ts8: e.tensor_tensor(out=G8[gi][:], in0=G8[gi][:], in1=RTT[:, 2, ts8].unsqueeze(2).broadcast_to([128, 8, 128]), op=ALU.mult), reads=rtt_ids + [("G8", gi)], writes=[("G8", gi)])
                        P.dve(lambda e, gi=gi, ts8=ts8, qr=qr: e.tensor_tensor(out=R8[gi][:], in0=CONST[:, C_IOTA + 32 * qr:C_IOTA + 32 * qr + 32].unsqueeze(1).broadcast_to([128, 8, 32]),
                                                                              in1=RTT[:, 0, ts8].unsqueeze(2).broadcast_to([128, 8, 32]), op=ALU.is_equal), reads=rtt_ids + ["CONST"], writes=[("R8", gi)])
                        for j in range(8):
                            P.pe(lambda e, gi=gi, j=j: e.matmul(PS[5][:, j * 32:(j + 1) * 32], lhsT=G8[gi][:, j, :], rhs=R8[gi][:, j, :], start=True, stop=True),
                                 reads=[("G8", gi), ("R8", gi)], writes=[("ps", 5)])
                        P.act(lambda e, ts8=ts8: e.activation(out=WT[:, :, ts8], in_=PS[5][:, 0:256].rearrange("p (t r) -> p r t", t=8), func=AF.Copy), reads=[("ps", 5)], writes=["WT"])
                    if qr == 0:
                        tap("d_wt", WT[:], ["WT"])
                    for g in range(8):
                        grp = []
                        for jj in range(4):
                            rl = g * 4 + jj
                            r = qr * 32 + rl
                            ub, ubid = stream_w(ut, 16, r * 128)
                            si, vi = wst_rot.nxt(), vb_rot.nxt()
                            P.dma(WST[si][:].rearrange("p k n -> p (k n)"), ev[r * 128:(r + 1) * 128, :], writes=[("WST", si)])
                            P.pool(lambda e, si=si, vi=vi: e.tensor_copy(out=VB[vi][:], in_=WST[si][:].rearrange("p k n -> p (k n)")), reads=[("WST", si)], writes=[("VB", vi)])
                            bi = ps_rot.nxt()
                            for k in range(16):
                                P.pe(lambda e, bi=bi, ub=ub, k=k: e.matmul(PS[bi][:], lhsT=ub[:, k, :], rhs=XG[:, k, :], start=(k == 0), stop=(k == 15)),
                                     reads=[ubid] + h2ids, writes=[("ps", bi)])
                            gi, pi = ga_rot.nxt(), pp_rot.nxt()
                            P.act(lambda e, bi=bi, gi=gi: e.activation(out=GA[gi][:], in_=PS[bi][:], func=AF.Gelu), reads=[("ps", bi)], writes=[("GA", gi)])
                            P.pool(lambda e, gi=gi, pi=pi, rl=rl: e.tensor_tensor(out=PP[pi][:], in0=GA[gi][:], in1=WT[:, rl, :], op=ALU.mult), reads=[("GA", gi), "WT"], writes=[("PP", pi)])
                            grp.append((vi, pi))
                        for n in range(16):
                            bi = 6 + ops_rot.nxt()
                            for jj, (vi, pi) in enumerate(grp):
                                P.pe(lambda e, bi=bi, vi=vi, pi=pi, jj=jj, n=n: e.matmul(PS[bi][:], lhsT=VB[vi][:, n * 128:(n + 1) * 128], rhs=PP[pi][:], start=(jj == 0), stop=(jj == 3)),
                                     reads=[("VB", vi), ("PP", pi)], writes=[("ps", bi)])
                            P.dve(lambda e, bi=bi, n=n: e.tensor_tensor(out=X32[:, n, :], in0=X32[:, n, :], in1=PS[bi][:], op=ALU.add), reads=[("ps", bi), ("X32", n // 4)], writes=[("X32", n // 4)])

                tap("d_xpre", X32[:], [("X32", q) for q in range(4)])
                for q in range(4):
                    P.act(lambda e, q=q: e.activation(out=SQ[:, 4 * q:4 * q + 4, :], in_=X32[:, 4 * q:4 * q + 4, :], func=AF.Square), reads=[("X32", q)], writes=[("SQ", q)])
                rms_from_sq(SQ, 16, [("SQ", q) for q in range(4)], D, RSTD, "RSTD", None, None)
                for k in range(16):
                    P.dve(lambda e, k=k: e.scalar_tensor_tensor(out=X32[:, k, :], in0=X32[:, k, :], scalar=CONST[:, C_GF + k:C_GF + k + 1], in1=RSTD[:], op0=ALU.mult, op1=ALU.mult),
                          reads=[("X32", k // 4), "RSTD", "CONST"], writes=[("X32", k // 4)])
                for q in range(4):
                    st = P.dma(outT[q * 512:(q + 1) * 512, t0:t0 + T].rearrange("(k p) t -> p k t", p=128), X32[:, 4 * q:4 * q + 4, :],
                               reads=[("X32", q)], writes=[("out", s, q)], semkey=("ost", q))
                    final_stores.append(st)
                P.barrier()

    for s_ in range(NSLOT):
        run_slot(s_)
    P.finish(final_waits=final_stores)
    es.close()
    return nc


OFF_CQ, OFF_CKV, OFF_KR, OFF_DQ, OFF_DK, OFF_DV, OFF_GATE = 0, 768, 1280, 1344, 2368, 3392, 4416


def _perm64(cols):
    cols = np.asarray(cols).reshape(-1, 64)
    return np.concatenate([cols[:, 32:], cols[:, :32]], axis=1).reshape(-1)


def _rope_tables(pos):
    inv = (10000.0 ** (-np.arange(0, 64, 2, dtype=np.float32) / np.float32(64))).astype(np.float32)
    ang = (pos.astype(np.float32)[None, :] * inv[:, None]).astype(np.float32)
    cos = np.cos(ang.astype(np.float64)).astype(np.float32)
    sin = np.sin(ang.astype(np.float64)).astype(np.float32)
    cos64 = np.concatenate([cos, cos], 0)
    sin64 = np.concatenate([-sin, sin], 0)
    return np.ascontiguousarray(np.stack([np.concatenate([cos64, cos64], 0), np.concatenate([sin64, sin64], 0)], 0))


_NC_CACHE = {}


def kernel(x, w_in, b_gate, g_norm1, g_cq, w_uq, g_ckv, w_ukv, w_o_mla, lambda_qk, g_subln, w_o_diff,
           w_out, g_norm2, w_q_peer, sub_keys, expert_u, expert_v, g_final):
    f = lambda a: np.asarray(a, dtype=np.float32)
    x = f(x)
    w_in0 = f(w_in)[0]
    ar = np.arange
    ckv = ar(OFF_CKV, OFF_KR)
    kr = ar(OFF_KR, OFF_DQ)
    kv_cols = [ckv, kr, kr, _perm64(kr), _perm64(kr)]
    for h in range(8):
        dk = ar(OFF_DK + h * 128, OFF_DK + (h + 1) * 128)
        kv_cols += [dk, _perm64(dk)]
    W_kv = np.ascontiguousarray(w_in0[:, np.concatenate(kv_cols)])
    W_dv = np.ascontiguousarray(w_in0[:, OFF_DV:OFF_GATE])
    q_cols = [ar(OFF_CQ, OFF_CKV)]
    for h in range(8):
        dq = ar(OFF_DQ + h * 128, OFF_DQ + (h + 1) * 128)
        q_cols += [dq, _perm64(dq)]
    W_q = np.ascontiguousarray(w_in0[:, np.concatenate(q_cols)])
    W_gate = np.ascontiguousarray(w_in0[:, OFF_GATE:])
    wuq = f(w_uq)[0]
    nope = np.concatenate([ar(h * 192, h * 192 + 128) for h in range(8)])
    rope = np.concatenate([ar(h * 192 + 128, h * 192 + 192) for h in range(8)])
    W_uq2 = np.ascontiguousarray(wuq[:, np.concatenate([nope, rope, _perm64(rope)])])
    wukv = f(w_ukv)[0]
    W_uk = np.ascontiguousarray(wukv[:, np.concatenate([ar(h * 256, h * 256 + 128) for h in range(8)])])
    W_uv = np.ascontiguousarray(wukv[:, np.concatenate([ar(h * 256 + 128, h * 256 + 256) for h in range(8)])])
    skT = np.ascontiguousarray(f(sub_keys)[0].transpose(0, 2, 1))
    ut = np.ascontiguousarray(f(expert_u)[0].T)
    ev = np.ascontiguousarray(f(expert_v)[0])
    col = lambda v: np.ascontiguousarray(f(v).reshape(-1, 128).T)
    consts = np.zeros((128, NC_CONST), np.float32)
    consts[:, C_G1:C_G1 + 16] = col(g_norm1[0])
    consts[:, C_GCQ:C_GCQ + 6] = col(g_cq[0])
    consts[:, C_GCKV:C_GCKV + 4] = col(g_ckv[0])
    consts[:, C_BG:C_BG + 32] = col(b_gate[0])
    consts[:, C_GSUB:C_GSUB + 1] = col(g_subln[0])
    consts[:, C_G2:C_G2 + 16] = col(g_norm2[0])
    consts[:, C_GF:C_GF + 16] = col(g_final)
    consts[:, C_LAM:C_LAM + 256] = f(lambda_qk)[0].reshape(1, 256)
    consts[:, C_KPOS:C_KPOS + 64] = (ar(64)[None, :] * 128 + ar(128)[:, None]).astype(np.float32)
    consts[:, C_IOTA:C_IOTA + 128] = ar(128, dtype=np.float32)[None, :]
    consts[:, C_IDENT:C_IDENT + 128] = np.eye(128, dtype=np.float32)
    cs_seq = _rope_tables(ar(SEQ))
    shared = {"consts": consts, "cs_seq": cs_seq, "w_kv": W_kv, "w_dv": W_dv, "w_q": W_q, "w_gate": W_gate,
              "w_uq2": W_uq2, "w_uk": W_uk, "w_uv": W_uv, "w_oa": np.ascontiguousarray(f(w_o_mla)[0]),
              "w_ob": np.ascontiguousarray(f(w_o_diff)[0]), "w_out": np.ascontiguousarray(f(w_out)[0]),
              "w_qp": np.ascontiguousarray(f(w_q_peer)[0]), "skT": skT, "ut": ut, "ev": ev}
    in_maps, own_pos = [], []
    xT = [np.ascontiguousarray(x[b].T) for b in range(2)]
    for c in range(8):
        b, p = c // 4, c % 4
        groups = [p, 7 - p, 8 + p, 15 - p]
        pos = np.concatenate([ar(g * 512, (g + 1) * 512) for g in groups])
        own_pos.append((b, pos))
        m = dict(shared)
        m["xT_seq"] = xT[b]
        m["xT_own"] = np.ascontiguousarray(xT[b][:, pos])
        m["cs_own"] = _rope_tables(pos)
        m["qpos"] = np.ascontiguousarray(np.broadcast_to(pos.astype(np.float32)[None, :], (128, 2048)))
        in_maps.append(m)
    if "nc" not in _NC_CACHE:
        _NC_CACHE["nc"] = build_program()
    res = run_bass_kernel_spmd(_NC_CACHE["nc"], in_maps, core_ids=list(range(8)))
    if DEBUG:
        _DBG["res"] = res.results
    out = np.zeros((2, SEQ, D), np.float32)
    for c in range(8):
        b, pos = own_pos[c]
        out[b, pos, :] = res.results[c]["outT"].T
    return out
```

```python
import math
from contextlib import ExitStack
import numpy as np
import concourse.bass as bass
import concourse.mybir as mybir
from concourse.bass_utils import run_bass_kernel_spmd

F32 = mybir.dt.float32
BF16 = mybir.dt.bfloat16
ALU = mybir.AluOpType
AF = mybir.ActivationFunctionType
ENGS = ("pe", "act", "dve", "pool", "sp")

D = 2048
SEQ = 8192
T = 512
EPS = 1e-6
NSLOT = 4
NE = 16384
DEBUG = False
_DBG = {}


class _Ins:
    __slots__ = ("eng", "fn", "deps", "dma", "semkey", "val", "signal")

    def __init__(self, eng, fn, dma, semkey):
        self.eng = eng
        self.fn = fn
        self.dma = dma
        self.semkey = semkey
        self.deps = set()
        self.val = None
        self.signal = False


class Prog:
    def __init__(self, nc):
        self.nc = nc
        self.ins = []
        self.last_write = {}
        self.readers = {}
        self.dma_counts = {}
        self.dma_last = {}
        self.eng_last = {}
        self.epoch = set()

    def op(self, eng, fn, reads=(), writes=(), dma=False, semkey=None):
        if dma and semkey is None:
            semkey = ("auto", writes[0])
        i = _Ins(eng, fn, dma, semkey)
        deps = set(self.epoch)
        for b in reads:
            w = self.last_write.get(b)
            if w is not None:
                deps.add(w)
        for b in writes:
            w = self.last_write.get(b)
            if w is not None:
                deps.add(w)
            for r in self.readers.get(b, ()):
                deps.add(r)
        for b in reads:
            self.readers.setdefault(b, []).append(i)
        for b in writes:
            self.last_write[b] = i
            self.readers[b] = []
        if dma:
            c = self.dma_counts.get(semkey, 0) + 16
            self.dma_counts[semkey] = c
            i.val = c
            self.dma_last[semkey] = i
        else:
            self.eng_last[eng] = i
        i.deps = deps
        self.ins.append(i)
        return i

    def barrier(self):
        self.epoch = set(self.eng_last.values()) | set(self.dma_last.values())
        self.last_write = {}
        self.readers = {}

    def pe(self, fn, reads=(), writes=()):
        return self.op("pe", fn, reads, writes)

    def act(self, fn, reads=(), writes=()):
        return self.op("act", fn, reads, writes)

    def dve(self, fn, reads=(), writes=()):
        return self.op("dve", fn, reads, writes)

    def pool(self, fn, reads=(), writes=()):
        return self.op("pool", fn, reads, writes)

    def dma(self, out, in_, reads=(), writes=(), semkey=None):
        return self.op("sp", lambda e: e.dma_start(out=out, in_=in_), reads, writes,
                       dma=True, semkey=semkey)

    def finish(self, final_waits=()):
        nc = self.nc
        for i in self.ins:
            if i.eng == "pe" and not i.dma:
                i.deps = {d for d in i.deps if not (d.eng == "pe" and not d.dma)}
        for i in self.ins:
            for d in i.deps:
                d.signal = True
        for d in final_waits:
            d.signal = True
        cnt = {e: 0 for e in ENGS}
        for i in self.ins:
            if not i.dma and i.signal:
                cnt[i.eng] += 1
                i.val = cnt[i.eng]
        with ExitStack() as es:
            esem = {e: es.enter_context(nc.semaphore("s_" + e)) for e in ENGS}
            dsem = {}
            for k in self.dma_counts:
                dsem[k] = es.enter_context(nc.semaphore("d%d" % len(dsem)))

            def tok(d):
                return (dsem[d.semkey] if d.dma else esem[d.eng]), d.val

            per = {e: [] for e in ENGS}
            for i in self.ins:
                per[i.eng].append(i)
            block = es.enter_context(nc.Block())

            def emit(engname, eh):
                waited = {}
                for i in per[engname]:
                    ws = {}
                    for d in i.deps:
                        s, v = tok(d)
                        key = id(s)
                        if waited.get(key, 0) >= v:
                            continue
                        if key not in ws or ws[key][1] < v:
                            ws[key] = (s, v)
                    for key, (s, v) in ws.items():
                        eh.wait_ge(s, v)
                        waited[key] = v
                    r = i.fn(eh)
                    if i.dma:
                        r.then_inc(dsem[i.semkey], 16)
                    elif i.signal:
                        r.then_inc(esem[i.eng], 1)
                if engname == "sp":
                    for d in final_waits:
                        s, v = tok(d)
                        eh.wait_ge(s, v)

            @block.tensor
            def _(e):
                emit("pe", e)

            @block.scalar
            def _(e):
                emit("act", e)

            @block.vector
            def _(e):
                emit("dve", e)

            @block.gpsimd
            def _(e):
                emit("pool", e)

            @block.sync
            def _(e):
                emit("sp", e)
        return cnt


class Rot:
    def __init__(self, n):
        self.n = n
        self.i = 0

    def nxt(self):
        r = self.i
        self.i = (self.i + 1) % self.n
        return r


C_G1, C_GCQ, C_GCKV, C_BG, C_GSUB, C_G2, C_GF = 0, 16, 22, 26, 58, 59, 75
C_LAM = 91
C_KPOS = 347
C_IOTA = 411
C_IDENT = 539
NC_CONST = 667

NKV = 22
NQ = 22


def build_program():
    nc = bass.Bass("TRN2", target_bir_lowering=False)
    dt_in = lambda n, s: nc.dram_tensor(n, s, F32, kind="ExternalInput").ap()
    xT_seq = dt_in("xT_seq", [D, SEQ])
    xT_own = dt_in("xT_own", [D, 2048])
    cs_seq = dt_in("cs_seq", [2, 128, SEQ])
    cs_own = dt_in("cs_own", [2, 128, 2048])
    qpos_d = dt_in("qpos", [128, 2048])
    consts_d = dt_in("consts", [128, NC_CONST])
    w_kv = dt_in("w_kv", [D, NKV * 128])
    w_dv = dt_in("w_dv", [D, 1024])
    w_q = dt_in("w_q", [D, NQ * 128])
    w_gate = dt_in("w_gate", [D, 4096])
    w_uq2 = dt_in("w_uq2", [768, 2048])
    w_uk = dt_in("w_uk", [512, 1024])
    w_uv = dt_in("w_uv", [512, 1024])
    w_oa = dt_in("w_oa", [1024, D])
    w_ob = dt_in("w_ob", [1024, D])
    w_out = dt_in("w_out", [D, D])
    w_qp = dt_in("w_qp", [D, D])
    skT = dt_in("skT", [2, 128, 128])
    ut = dt_in("ut", [D, NE])
    ev = dt_in("ev", [NE, D])
    outT = nc.dram_tensor("outT", [D, 2048], F32, kind="ExternalOutput").ap()
    skind = "ExternalOutput" if DEBUG else "Internal"
    kTm = nc.dram_tensor("kTm", [8, 128, SEQ], BF16, kind=skind).ap()
    kTr = nc.dram_tensor("kTr", [128, SEQ], BF16, kind=skind).ap()
    kTd = nc.dram_tensor("kTd", [8, 128, SEQ], BF16, kind=skind).ap()
    vM = nc.dram_tensor("vM", [SEQ, 1024], BF16, kind=skind).ap()
    vD = nc.dram_tensor("vD", [SEQ, 1024], BF16, kind=skind).ap()
    taps = {}
    if DEBUG:
        for nm, shp, dtp in (("d_x1", [128, 16, T], F32), ("d_otm", [128, 8, T], BF16), ("d_otd", [128, 8, T], BF16),
                             ("d_qtm", [128, 8, T], BF16), ("d_qtr", [128, 4, T], BF16), ("d_qtd", [128, 8, T], BF16),
                             ("d_h2", [128, 16, T], BF16), ("d_rtt", [128, 3, T], F32), ("d_qp", [128, 16, T], BF16),
                             ("d_xpre", [128, 16, T], F32), ("d_wt", [128, 32, T], BF16)):
            taps[nm] = nc.dram_tensor(nm, shp, dtp, kind="ExternalOutput").ap()

    def tap(nm, tile_ap, ids):
        if DEBUG and nm in taps:
            final_stores.append(P.dma(taps.pop(nm), tile_ap, reads=ids, writes=[("tap", nm)], semkey=("tap", nm)))


    final_stores = []
    es = ExitStack()
    sb = lambda n, s, d: es.enter_context(nc.sbuf_tensor(n, s, d))
    PS = [es.enter_context(nc.psum_tensor("ps%d" % i, [128, 512], F32)) for i in range(8)]
    P = Prog(nc)

    CONST = sb("CONST", [128, NC_CONST], F32)
    ONES = sb("ONES", [128, 128], BF16)
    IDB = sb("IDB", [128, 128], BF16)
    SMALL = sb("SMALL", [128, 16], F32)
    LAMT = sb("LAMT", [128, 128], F32)
    P.dma(CONST[:], consts_d, writes=["CONST"])
    P.pool(lambda e: e.memset(ONES[:], 1.0), writes=["ONES"])
    P.dve(lambda e: e.tensor_copy(out=IDB[:], in_=CONST[:, C_IDENT:C_IDENT + 128]), reads=["CONST"], writes=["IDB"])
    lam_init = 0.8 - 0.6 * math.exp(-0.3 * 0)
    P.dve(lambda e: e.tensor_tensor(out=LAMT[:, 0:64], in0=CONST[:, C_LAM:C_LAM + 64], in1=CONST[:, C_LAM + 64:C_LAM + 128], op=ALU.mult), reads=["CONST"], writes=["LAMT0"])
    P.dve(lambda e: e.tensor_tensor(out=LAMT[:, 64:128], in0=CONST[:, C_LAM + 128:C_LAM + 192], in1=CONST[:, C_LAM + 192:C_LAM + 256], op=ALU.mult), reads=["CONST"], writes=["LAMT1"])
    P.dve(lambda e: e.reduce_sum(out=SMALL[:, 2:3], in_=LAMT[:, 0:64], axis=mybir.AxisListType.X), reads=["LAMT0"], writes=["SM2"])
    P.dve(lambda e: e.reduce_sum(out=SMALL[:, 3:4], in_=LAMT[:, 64:128], axis=mybir.AxisListType.X), reads=["LAMT1"], writes=["SM3"])
    P.act(lambda e: e.activation(out=SMALL[:, 4:6], in_=SMALL[:, 2:4], func=AF.Exp), reads=["SM2", "SM3"], writes=["SM45"])
    P.dve(lambda e: e.tensor_tensor(out=SMALL[:, 6:7], in0=SMALL[:, 5:6], in1=SMALL[:, 4:5], op=ALU.subtract), reads=["SM45"], writes=["SM6"])
    P.dve(lambda e: e.tensor_scalar(out=SMALL[:, 0:1], in0=SMALL[:, 6:7], scalar1=-lam_init, scalar2=None, op0=ALU.add), reads=["SM6"], writes=["NEGLAM"])
    P.dve(lambda e: e.tensor_scalar(out=SMALL[:, 1:2], in0=CONST[:, C_GSUB:C_GSUB + 1], scalar1=1.0 - lam_init, scalar2=None, op0=ALU.mult), reads=["CONST"], writes=["GSUBS"])

    XG = sb("XG", [128, 16, T], BF16)
    RSTD = sb("RSTD", [128, T], F32)
    RSTDT = sb("RSTDT", [128, 8], F32)
    TMPS = sb("TMPS", [128, T], F32)
    WST = [sb("WST%d" % i, [128, 16, 128], F32) for i in range(3)]
    WB = [sb("WB%d" % i, [128, 16, 128], BF16) for i in range(3)]
    wst_rot, wb_rot, ps_rot, cast_rot = Rot(3), Rot(3), Rot(2), Rot(3)

    def stream_w(src, kc, col0, ncols=128):
        si, bi = wst_rot.nxt(), wb_rot.nxt()
        st, wb = WST[si], WB[bi]
        P.dma(st[:, 0:kc, 0:ncols], src[0:kc * 128, col0:col0 + ncols].rearrange("(k p) n -> p k n", p=128),
              writes=[("WST", si)])
        if cast_rot.nxt() == 2:
            P.dve(lambda e: e.tensor_copy(out=wb[:, 0:kc, 0:ncols], in_=st[:, 0:kc, 0:ncols]), reads=[("WST", si)], writes=[("WB", bi)])
        else:
            P.act(lambda e: e.activation(out=wb[:, 0:kc, 0:ncols], in_=st[:, 0:kc, 0:ncols], func=AF.Copy), reads=[("WST", si)], writes=[("WB", bi)])
        return wb, ("WB", bi)

    def linear_chunk(src, kc, col0, rhs_fn, rhs_ids, pbank=None):
        wb, wid = stream_w(src, kc, col0)
        bi = ps_rot.nxt() if pbank is None else pbank
        ps = PS[bi]
        for k in range(kc):
            P.pe(lambda e, k=k: e.matmul(ps[:], lhsT=wb[:, k, :], rhs=rhs_fn(k), start=(k == 0), stop=(k == kc - 1)),
                 reads=[wid] + rhs_ids, writes=[("ps", bi)])
        return ps, ("ps", bi)

    def load_x_and_norm(X32, SQ, src, t0, gcol, tokmajor):
        for q in range(4):
            P.dma(X32[:, 4 * q:4 * q + 4, :], src[q * 512:(q + 1) * 512, t0:t0 + T].rearrange("(k p) t -> p k t", p=128),
                  writes=[("X32", q)])
        x32ids = [("X32", q) for q in range(4)]
        for q in range(4):
            P.act(lambda e, q=q: e.activation(out=SQ[:, 4 * q:4 * q + 4, :], in_=X32[:, 4 * q:4 * q + 4, :], func=AF.Square),
                  reads=[("X32", q)], writes=[("SQ", q)])
        sqids = [("SQ", q) for q in range(4)]
        rms_from_sq(SQ, 16, sqids, D, RSTD, "RSTD", RSTDT if tokmajor else None, "RSTDT")
        for k in range(16):
            P.act(lambda e, k=k: e.activation(out=XG[:, k, :], in_=X32[:, k, :], func=AF.Copy, scale=CONST[:, gcol + k:gcol + k + 1]),
                  reads=[("X32", k // 4), "CONST"], writes=[("XG", k)])
        return x32ids, [("XG", k) for k in range(16)]

    def rms_from_sq(sq, kc, sqids, dim, rstd, rid, rstdt, rtid):
        ps = PS[2]
        for k in range(kc):
            P.pe(lambda e, k=k: e.matmul(ps[:], lhsT=ONES[:], rhs=sq[:, k, :], start=(k == 0), stop=(k == kc - 1)),
                 reads=["ONES"] + sqids, writes=[("ps", 2)])
        P.act(lambda e: e.activation(out=rstd[:], in_=ps[:], func=AF.Sqrt, scale=1.0 / dim, bias=EPS), reads=[("ps", 2)], writes=[rid + "s"])
        P.dve(lambda e: e.reciprocal(out=rstd[:], in_=rstd[:]), reads=[rid + "s"], writes=[rid])
        if rstdt is not None:
            ps3 = PS[3]
            for j in range(4):
                for k in range(kc):
                    P.pe(lambda e, k=k, j=j: e.matmul(ps3[:, j:j + 1], lhsT=sq[:, k, j * 128:(j + 1) * 128], rhs=ONES[:, 0:1], start=(k == 0), stop=(k == kc - 1)),
                         reads=["ONES"] + sqids, writes=[("ps", 3)])
            P.act(lambda e: e.activation(out=rstdt[:, 0:4], in_=ps3[:, 0:4], func=AF.Sqrt, scale=1.0 / dim, bias=EPS), reads=[("ps", 3)], writes=[rtid + "s"])
            P.dve(lambda e: e.reciprocal(out=rstdt[:, 0:4], in_=rstdt[:, 0:4]), reads=[rtid + "s"], writes=[rtid])

    def run_phase1():
        with ExitStack() as es1:
            sb1 = lambda n, s, d: es1.enter_context(nc.sbuf_tensor(n, s, d))
            X32 = sb1("X32p1", [128, 16, T], F32)
            SQ = sb1("SQp1", [128, 16, T], BF16)
            WDV = sb1("WDV", [128, 16, 1024], BF16)
            WUK = sb1("WUK", [128, 4, 1024], BF16)
            WUV = sb1("WUV", [128, 4, 1024], BF16)
            CKV32 = sb1("CKV32", [128, 4, T], F32)
            SQ2 = sb1("SQ2", [128, 4, T], BF16)
            RSTD2 = sb1("RSTD2", [128, T], F32)
            CKVN = sb1("CKVN", [128, 4, T], BF16)
            CSK = sb1("CSK", [128, 2, T], F32)
            RA = sb1("RA", [128, T], F32)
            RB = sb1("RB", [128, T], F32)
            KOUT = [sb1("KOUT%d" % i, [128, T], BF16) for i in range(2)]
            VOUT = [sb1("VOUT%d" % i, [128, 1024], BF16) for i in range(2)]
            ko_rot, vo_rot, vps_rot = Rot(2), Rot(2), Rot(2)
            for c8 in range(8):
                wb, wid = stream_w(w_dv, 16, c8 * 128)
                P.pool(lambda e, wb=wb, c8=c8: e.tensor_copy(out=WDV[:, :, c8 * 128:(c8 + 1) * 128], in_=wb[:, :, :]), reads=[wid], writes=[("WDV", c8)])
                wb, wid = stream_w(w_uk, 4, c8 * 128)
                P.pool(lambda e, wb=wb, c8=c8: e.tensor_copy(out=WUK[:, :, c8 * 128:(c8 + 1) * 128], in_=wb[:, 0:4, :]), reads=[wid], writes=[("WUK", c8)])
                wb, wid = stream_w(w_uv, 4, c8 * 128)
                P.pool(lambda e, wb=wb, c8=c8: e.tensor_copy(out=WUV[:, :, c8 * 128:(c8 + 1) * 128], in_=wb[:, 0:4, :]), reads=[wid], writes=[("WUV", c8)])
            wdv_ids = [("WDV", c) for c in range(8)]
            wuk_ids = [("WUK", c) for c in range(8)]
            wuv_ids = [("WUV", c) for c in range(8)]

            def rope_store(psA_fn, psB_fn, dst):
                psA, idA = psA_fn()
                P.dve(lambda e: e.tensor_tensor(out=RA[:], in0=psA[:], in1=CSK[:, 0, :], op=ALU.mult), reads=[idA, "CSKR"], writes=["RA"])
                psB, idB = psB_fn()
                P.dve(lambda e: e.tensor_tensor(out=RB[:], in0=psB[:], in1=CSK[:, 1, :], op=ALU.mult), reads=[idB, "CSKR"], writes=["RB"])
                ki = ko_rot.nxt()
                P.pool(lambda e: e.tensor_tensor(out=KOUT[ki][:], in0=RA[:], in1=RB[:], op=ALU.add), reads=["RA", "RB"], writes=[("KOUT", ki)])
                P.dma(dst, KOUT[ki][:], reads=[("KOUT", ki)], writes=[("kscr", ki)], semkey=("kst", ki))

            for it in range(SEQ // T):
                t0 = it * T
                x32ids, xgids = load_x_and_norm(X32, SQ, xT_seq, t0, C_G1, True)
                P.dma(CSK[:], cs_seq[:, :, t0:t0 + T].rearrange("c p t -> p c t"), writes=["CSK", "CSKR"])
                for c2 in range(2):
                    P.dve(lambda e, c2=c2: e.tensor_tensor(out=CSK[:, c2, :], in0=CSK[:, c2, :], in1=RSTD[:], op=ALU.mult), reads=["CSK", "RSTD"], writes=["CSKR"])
                rhs_x = lambda k: XG[:, k, :]
                for n in range(4):
                    ps, pid = linear_chunk(w_kv, 16, n * 128, rhs_x, xgids)
                    P.dve(lambda e, ps=ps, n=n: e.tensor_tensor(out=CKV32[:, n, :], in0=ps[:], in1=RSTD[:], op=ALU.mult), reads=[pid, "RSTD"], writes=[("CKV32", n)])
                    P.act(lambda e, n=n: e.activation(out=SQ2[:, n, :], in_=CKV32[:, n, :], func=AF.Square), reads=[("CKV32", n)], writes=[("SQ2", n)])
                rope_store(lambda: linear_chunk(w_kv, 16, 4 * 128, rhs_x, xgids), lambda: linear_chunk(w_kv, 16, 5 * 128, rhs_x, xgids), kTr[:, t0:t0 + T])
                for h in range(8):
                    rope_store(lambda h=h: linear_chunk(w_kv, 16, (6 + 2 * h) * 128, rhs_x, xgids),
                               lambda h=h: linear_chunk(w_kv, 16, (7 + 2 * h) * 128, rhs_x, xgids), kTd[h, :, t0:t0 + T])
                sq2ids = [("SQ2", n) for n in range(4)]
                rms_from_sq(SQ2, 4, sq2ids, 512, RSTD2, "RSTD2", None, None)
                for n in range(4):
                    P.dve(lambda e, n=n: e.scalar_tensor_tensor(out=CKVN[:, n, :], in0=CKV32[:, n, :], scalar=CONST[:, C_GCKV + n:C_GCKV + n + 1], in1=RSTD2[:], op0=ALU.mult, op1=ALU.mult),
                          reads=[("CKV32", n), "RSTD2", "CONST"], writes=[("CKVN", n)])
                ckvn_ids = [("CKVN", n) for n in range(4)]
                for h in range(8):
                    bi = ps_rot.nxt()
                    ps = PS[bi]
                    for j in range(4):
                        P.pe(lambda e, ps=ps, j=j, h=h: e.matmul(ps[:], lhsT=WUK[:, j, h * 128:(h + 1) * 128], rhs=CKVN[:, j, :], start=(j == 0), stop=(j == 3)),
                             reads=wuk_ids + ckvn_ids, writes=[("ps", bi)])
                    ki = ko_rot.nxt()
                    P.act(lambda e, ps=ps, ki=ki: e.activation(out=KOUT[ki][:], in_=ps[:], func=AF.Copy), reads=[("ps", bi)], writes=[("KOUT", ki)])
                    P.dma(kTm[h, :, t0:t0 + T], KOUT[ki][:], reads=[("KOUT", ki)], writes=[("kscr", ki)], semkey=("kst", ki))
                for j4 in range(4):
                    vi = vo_rot.nxt()
                    for half in range(2):
                        bi = 4 + vps_rot.nxt()
                        ps = PS[bi]
                        for j in range(4):
                            P.pe(lambda e, ps=ps, j=j, j4=j4, half=half: e.matmul(ps[:], lhsT=CKVN[:, j, j4 * 128:(j4 + 1) * 128], rhs=WUV[:, j, half * 512:(half + 1) * 512], start=(j == 0), stop=(j == 3)),
                                 reads=wuv_ids + ckvn_ids, writes=[("ps", bi)])
                        P.act(lambda e, ps=ps, vi=vi, half=half: e.activation(out=VOUT[vi][:, half * 512:(half + 1) * 512], in_=ps[:], func=AF.Copy), reads=[("ps", bi)], writes=[("VOUT", vi, half)])
                    P.dma(vM[t0 + j4 * 128:t0 + (j4 + 1) * 128, :], VOUT[vi][:], reads=[("VOUT", vi, 0), ("VOUT", vi, 1)], writes=[("vscr", vi)], semkey=("vst", vi))
                    vi = vo_rot.nxt()
                    for half in range(2):
                        bi = 4 + vps_rot.nxt()
                        ps = PS[bi]
                        for k in range(16):
                            P.pe(lambda e, ps=ps, k=k, j4=j4, half=half: e.matmul(ps[:], lhsT=XG[:, k, j4 * 128:(j4 + 1) * 128], rhs=WDV[:, k, half * 512:(half + 1) * 512], start=(k == 0), stop=(k == 15)),
                                 reads=wdv_ids + xgids, writes=[("ps", bi)])
                        P.act(lambda e, ps=ps, vi=vi, half=half, j4=j4: e.activation(out=VOUT[vi][:, half * 512:(half + 1) * 512], in_=ps[:], func=AF.Copy, scale=RSTDT[:, j4:j4 + 1]),
                              reads=[("ps", bi), "RSTDT"], writes=[("VOUT", vi, half)])
                    P.dma(vD[t0 + j4 * 128:t0 + (j4 + 1) * 128, :], VOUT[vi][:], reads=[("VOUT", vi, 0), ("VOUT", vi, 1)], writes=[("vscr", vi)], semkey=("vst", vi))
            P.barrier()


    run_phase1()

    def run_slot(s):
        t0 = s * T
        nkt = 16 * (s + 1)
        with ExitStack() as esS:
            sbS = lambda n, sh, d: esS.enter_context(nc.sbuf_tensor("%s_%d" % (n, s), sh, d))
            X32 = sbS("X32", [128, 16, T], F32)
            SQ = sbS("SQ", [128, 16, T], BF16)
            with ExitStack() as esO:
                sbO = lambda n, sh, d: esO.enter_context(nc.sbuf_tensor("%s_%d" % (n, s), sh, d))
                OTM = sbO("OTM", [128, 8, T], BF16)
                OTD = sbO("OTD", [128, 8, T], BF16)
                with ExitStack() as esQ:
                    sbQ = lambda n, sh, d: esQ.enter_context(nc.sbuf_tensor("%s_%d" % (n, s), sh, d))
                    QTM = sbQ("QTM", [128, 8, T], BF16)
                    QTR = sbQ("QTR", [128, 4, T], BF16)
                    QTD = sbQ("QTD", [128, 8, T], BF16)
                    with ExitStack() as esA:
                        sbA = lambda n, sh, d: esA.enter_context(nc.sbuf_tensor("%s_%d" % (n, s), sh, d))
                        CQ32 = sbA("CQ32", [128, 6, T], F32)
                        SQ3 = sbA("SQ3", [128, 6, T], BF16)
                        CQN = sbA("CQN", [128, 6, T], BF16)
                        CSQ = sbA("CSQ", [128, 2, T], F32)
                        RA = sbA("RA2", [128, T], F32)
                        RB = sbA("RB2", [128, T], F32)
                        x32ids, xgids = load_x_and_norm(X32, SQ, xT_own, t0, C_G1, False)
                        P.dma(CSQ[:], cs_own[:, :, t0:t0 + T].rearrange("c p t -> p c t"), writes=["CSQ"])
                        rhs_x = lambda k: XG[:, k, :]

                        CSQR = sbA("CSQR", [128, 2, T], F32)
                        for c2 in range(2):
                            P.dve(lambda e, c2=c2: e.tensor_tensor(out=CSQR[:, c2, :], in0=CSQ[:, c2, :], in1=RSTD[:], op=ALU.mult), reads=["CSQ", "RSTD"], writes=[("CSQR", c2)])

                        def rope_q(psA_fn, psB_fn, dst, dst_id, scaled):
                            tab = CSQR if scaled else CSQ
                            tids = [("CSQR", 0), ("CSQR", 1)] if scaled else ["CSQ"]
                            psA, idA = psA_fn()
                            P.dve(lambda e: e.tensor_tensor(out=RA[:], in0=psA[:], in1=tab[:, 0, :], op=ALU.mult), reads=[idA] + tids, writes=["RA"])
                            psB, idB = psB_fn()
                            P.dve(lambda e: e.tensor_tensor(out=RB[:], in0=psB[:], in1=tab[:, 1, :], op=ALU.mult), reads=[idB] + tids, writes=["RB"])
                            P.pool(lambda e: e.tensor_tensor(out=dst, in0=RA[:], in1=RB[:], op=ALU.add), reads=["RA", "RB"], writes=[dst_id])

                        for n in range(6):
                            ps, pid = linear_chunk(w_q, 16, n * 128, rhs_x, xgids)
                            P.dve(lambda e, ps=ps, n=n: e.tensor_tensor(out=CQ32[:, n, :], in0=ps[:], in1=RSTD[:], op=ALU.mult), reads=[pid, "RSTD"], writes=[("CQ32", n)])
                            P.act(lambda e, n=n: e.activation(out=SQ3[:, n, :], in_=CQ32[:, n, :], func=AF.Square), reads=[("CQ32", n)], writes=[("SQ3", n)])
                        for h in range(8):
                            rope_q(lambda h=h: linear_chunk(w_q, 16, (6 + 2 * h) * 128, rhs_x, xgids),
                                   lambda h=h: linear_chunk(w_q, 16, (7 + 2 * h) * 128, rhs_x, xgids), QTD[:, h, :], ("QTD", h), True)
                        sq3ids = [("SQ3", n) for n in range(6)]
                        rms_from_sq(SQ3, 6, sq3ids, 768, TMPS, "TMPS", None, None)
                        for n in range(6):
                            P.dve(lambda e, n=n: e.scalar_tensor_tensor(out=CQN[:, n, :], in0=CQ32[:, n, :], scalar=CONST[:, C_GCQ + n:C_GCQ + n + 1], in1=TMPS[:], op0=ALU.mult, op1=ALU.mult),
                                  reads=[("CQ32", n), "TMPS", "CONST"], writes=[("CQN", n)])
                        cqn_ids = [("CQN", n) for n in range(6)]
                        rhs_c = lambda k: CQN[:, k, :]
                        for h in range(8):
                            ps, pid = linear_chunk(w_uq2, 6, h * 128, rhs_c, cqn_ids)
                            P.act(lambda e, ps=ps, h=h: e.activation(out=QTM[:, h, :], in_=ps[:], func=AF.Copy), reads=[pid], writes=[("QTM", h)])
                        for j in range(4):
                            rope_q(lambda j=j: linear_chunk(w_uq2, 6, (8 + j) * 128, rhs_c, cqn_ids),
                                   lambda j=j: linear_chunk(w_uq2, 6, (12 + j) * 128, rhs_c, cqn_ids), QTR[:, j, :], ("QTR", j), False)
                        tap("d_qtm", QTM[:], [("QTM", h) for h in range(8)])
                        tap("d_qtr", QTR[:], [("QTR", h) for h in range(4)])
                        tap("d_qtd", QTD[:], [("QTD", h) for h in range(8)])
                        P.barrier()
                    with ExitStack() as esB:
                        sbB = lambda n, sh, d: esB.enter_context(nc.sbuf_tensor("%s_%d" % (n, s), sh, d))
                        QPOS = sbB("QPOS", [128, T], F32)
                        KC = [sbB("KC%d" % i, [128, 2048], BF16) for i in range(3)]
                        KRC = [sbB("KRC%d" % i, [128, 2048], BF16) for i in range(3)]
                        VC = [sbB("VC%d" % i, [128, 16, 128], BF16) for i in range(3)]
                        PT = [sbB("PT%d" % i, [128, T], BF16) for i in range(3)]
                        A1 = sbB("A1", [128, T], F32)
                        A2 = sbB("A2", [128, T], F32)
                        RL = sbB("RL", [128, T], F32)
                        SQD = sbB("SQD", [128, 1, T], BF16)
                        kc_rot, pt_rot = Rot(3), Rot(3)
                        P.dma(QPOS[:], qpos_d[:, t0:t0 + T], writes=["QPOS"])

                        def attend(ksrc, vsrc, h, with_rope, streams):
                            for kt in range(nkt):
                                c16, kk = kt // 16, kt % 16
                                if kk == 0:
                                    ci = kc_rot.nxt()
                                    ks = slice(c16 * 2048, (c16 + 1) * 2048)
                                    P.dma(KC[ci][:], ksrc[h, :, ks], writes=[("KC", ci)])
                                    P.dma(VC[ci][:], vsrc[ks, h * 128:(h + 1) * 128].rearrange("(kt p) d -> p kt d", p=128), writes=[("VC", ci)])
                                    kids = [("KC", ci)]
                                    if with_rope:
                                        P.dma(KRC[ci][:], kTr[:, ks], writes=[("KRC", ci)])
                                        kids.append(("KRC", ci))
                                for (parts_fn, q_ids, scale, o_bank, l_bank) in streams:
                                    bi = ps_rot.nxt()
                                    ps = PS[bi]
                                    parts = parts_fn(ci, kk)
                                    for pi, (lh, rh) in enumerate(parts):
                                        P.pe(lambda e, ps=ps, lh=lh, rh=rh, pi=pi, n=len(parts): e.matmul(ps[:], lhsT=lh, rhs=rh, start=(pi == 0), stop=(pi == n - 1)),
                                             reads=kids + q_ids, writes=[("ps", bi)])
                                    pti = pt_rot.nxt()
                                    pt = PT[pti]
                                    P.act(lambda e, ps=ps, pt=pt, scale=scale: e.activation(out=pt[:], in_=ps[:], func=AF.Exp, scale=scale), reads=[("ps", bi)], writes=[("PT", pti)])
                                    if kt >= 16 * s:
                                        P.dve(lambda e, pt=pt, kt=kt: e.scalar_tensor_tensor(out=pt[:], in0=QPOS[:], scalar=CONST[:, C_KPOS + kt:C_KPOS + kt + 1], in1=pt[:], op0=ALU.is_ge, op1=ALU.mult),
                                              reads=[("PT", pti), "QPOS", "CONST"], writes=[("PT", pti)])
                                    P.pe(lambda e, pt=pt, kt=kt, ci=ci, kk=kk, o_bank=o_bank: e.matmul(PS[o_bank][:], lhsT=VC[ci][:, kk, :], rhs=pt[:], start=(kt == 0), stop=(kt == nkt - 1)),
                                         reads=[("PT", pti), ("VC", ci)], writes=[("ps", o_bank)])
                                    P.pe(lambda e, pt=pt, kt=kt, l_bank=l_bank: e.matmul(PS[l_bank][:], lhsT=ONES[:], rhs=pt[:], start=(kt == 0), stop=(kt == nkt - 1)),
                                         reads=[("PT", pti), "ONES"], writes=[("ps", l_bank)])

                        for h in range(8):
                            hb = 64 * (h % 2)
                            attend(kTm, vM, h, True,
                                   [(lambda ci, kk, h=h, hb=hb: [(KC[ci][:, kk * 128:(kk + 1) * 128], QTM[:, h, :]),
                                                                 (KRC[ci][hb:hb + 64, kk * 128:(kk + 1) * 128], QTR[hb:hb + 64, h // 2, :])],
                                     [("QTM", h), ("QTR", h // 2)], 192.0 ** -0.5, 2, 3)])
                            P.dve(lambda e: e.reciprocal(out=RL[:], in_=PS[3][:]), reads=[("ps", 3)], writes=["RL"])
                            P.dve(lambda e, h=h: e.tensor_tensor(out=OTM[:, h, :], in0=PS[2][:], in1=RL[:], op=ALU.mult), reads=[("ps", 2), "RL"], writes=[("OTM", h)])
                        for h in range(8):
                            attend(kTd, vD, h, False,
                                   [(lambda ci, kk, h=h, m=m: [(KC[ci][64 * m:64 * m + 64, kk * 128:(kk + 1) * 128], QTD[64 * m:64 * m + 64, h, :])],
                                     [("QTD", h)], 64.0 ** -0.5, 2 + 2 * m, 3 + 2 * m) for m in range(2)])
                            P.dve(lambda e: e.reciprocal(out=RL[:], in_=PS[3][:]), reads=[("ps", 3)], writes=["RL"])
                            P.dve(lambda e: e.tensor_tensor(out=A1[:], in0=PS[2][:], in1=RL[:], op=ALU.mult), reads=[("ps", 2), "RL"], writes=["A1"])
                            P.dve(lambda e: e.reciprocal(out=RL[:], in_=PS[5][:]), reads=[("ps", 5), "A1"], writes=["RL"])
                            P.dve(lambda e: e.scalar_tensor_tensor(out=A2[:], in0=PS[4][:], scalar=SMALL[:, 0:1], in1=RL[:], op0=ALU.mult, op1=ALU.mult), reads=[("ps", 4), "RL", "NEGLAM"], writes=["A2"])
                            P.pool(lambda e: e.tensor_tensor(out=A1[:], in0=A1[:], in1=A2[:], op=ALU.add), reads=["A1", "A2"], writes=["A1"])
                            P.act(lambda e: e.activation(out=SQD[:, 0, :], in_=A1[:], func=AF.Square), reads=["A1"], writes=["SQD"])
                            rms_from_sq(SQD, 1, ["SQD"], 128, A2, "A2", None, None)
                            P.dve(lambda e, h=h: e.scalar_tensor_tensor(out=OTD[:, h, :], in0=A1[:], scalar=SMALL[:, 1:2], in1=A2[:], op0=ALU.mult, op1=ALU.mult),
                                  reads=["A1", "A2", "GSUBS"], writes=[("OTD", h)])
                        tap("d_otm", OTM[:], [("OTM", h) for h in range(8)])
                        tap("d_otd", OTD[:], [("OTD", h) for h in range(8)])
                        P.barrier()
                with ExitStack() as esC:
                    sbC = lambda n, sh, d: esC.enter_context(nc.sbuf_tensor("%s_%d" % (n, s), sh, d))
                    MG = sbC("MG", [128, 16, T], BF16)
                    M1 = sbC("M1", [128, T], F32)
                    M2 = sbC("M2", [128, T], F32)
                    GT_ = sbC("GT_", [128, T], F32)
                    xgids = [("XG", k) for k in range(16)]
                    rhs_x = lambda k: XG[:, k, :]
                    otm_ids = [("OTM", h) for h in range(8)]
                    otd_ids = [("OTD", h) for h in range(8)]
                    for n in range(16):
                        for br, (wo, OT, oids, mdst, mid) in enumerate(((w_oa, OTM, otm_ids, M1, "M1"), (w_ob, OTD, otd_ids, M2, "M2"))):
                            psg, pgid = linear_chunk(w_gate, 16, (br * 16 + n) * 128, rhs_x, xgids)
                            P.dve(lambda e, psg=psg: e.tensor_tensor(out=GT_[:], in0=psg[:], in1=RSTD[:], op=ALU.mult), reads=[pgid, "RSTD"], writes=["GT_"])
                            P.act(lambda e, br=br, n=n: e.activation(out=GT_[:], in_=GT_[:], func=AF.Sigmoid, bias=CONST[:, C_BG + br * 16 + n:C_BG + br * 16 + n + 1]), reads=["GT_", "CONST"], writes=["GT_"])
                            psy, pyid = linear_chunk(wo, 8, n * 128, lambda k, OT=OT: OT[:, k, :], oids)
                            P.dve(lambda e, psy=psy, mdst=mdst: e.tensor_tensor(out=mdst[:], in0=psy[:], in1=GT_[:], op=ALU.mult), reads=[pyid, "GT_"], writes=[mid])
                        P.pool(lambda e, n=n: e.tensor_tensor(out=MG[:, n, :], in0=M1[:], in1=M2[:], op=ALU.add), reads=["M1", "M2"], writes=[("MG", n)])
                    mg_ids = [("MG", n) for n in range(16)]
                    for n in range(16):
                        ps, pid = linear_chunk(w_out, 16, n * 128, lambda k: MG[:, k, :], mg_ids)
                        P.dve(lambda e, ps=ps, n=n: e.tensor_tensor(out=X32[:, n, :], in0=X32[:, n, :], in1=ps[:], op=ALU.add), reads=[pid, ("X32", n // 4)], writes=[("X32", n // 4)])
                    tap("d_x1", X32[:], [("X32", q) for q in range(4)])
                    P.barrier()

            with ExitStack() as es3:
                sb3 = lambda n, sh, d: es3.enter_context(nc.sbuf_tensor("%s_%d" % (n, s), sh, d))
                QP = SQ
                SKB = sb3("SKB", [128, 2, 128], BF16)
                S32 = sb3("S32", [128, 16, 128], F32)
                TK = sb3("TK", [128, 2, 128], F32)
                V12 = sb3("V12", [128, 2, 16], F32)
                I1 = sb3("I1", [128, 2, 16], mybir.dt.uint32)
                I1F = sb3("I1F", [128, 2, 16], F32)
                CAND = sb3("CAND", [128, 256], F32)
                CAND2 = sb3("CAND2", [128, 256], F32)
                C16 = sb3("C16", [128, 16], F32)
                CI = sb3("CI", [128, 16], mybir.dt.uint32)
                CIH = sb3("CIH", [128, 16], mybir.dt.uint32)
                CIL = sb3("CIL", [128, 16], mybir.dt.uint32)
                CIHF = sb3("CIHF", [128, 16], F32)
                CILF = sb3("CILF", [128, 16], F32)
                OH = sb3("OH", [128, 16, 16], F32)
                SM = sb3("SM", [128, 8], F32)
                EJ = sb3("EJ", [128, 16], F32)
                ROWC = sb3("ROWC", [128, 3, 128], BF16)
                RTT = sb3("RTT", [128, 3, T], F32)
                WT = sb3("WT", [128, 32, T], BF16)
                G8 = [sb3("G8_%d" % i, [128, 8, 128], BF16) for i in range(2)]
                R8 = [sb3("R8_%d" % i, [128, 8, 32], BF16) for i in range(2)]
                VB = [sb3("VB%d" % i, [128, D], BF16) for i in range(6)]
                GA = [sb3("GA%d" % i, [128, T], BF16) for i in range(2)]
                PP = [sb3("PP%d" % i, [128, T], BF16) for i in range(5)]
                g8_rot, vb_rot, ga_rot, pp_rot, ops_rot = Rot(2), Rot(6), Rot(2), Rot(5), Rot(2)

                for q in range(4):
                    P.act(lambda e, q=q: e.activation(out=SQ[:, 4 * q:4 * q + 4, :], in_=X32[:, 4 * q:4 * q + 4, :], func=AF.Square), reads=[("X32", q)], writes=[("SQ", q)])
                rms_from_sq(SQ, 16, [("SQ", q) for q in range(4)], D, RSTD, "RSTD", None, None)
                for k in range(16):
                    P.dve(lambda e, k=k: e.scalar_tensor_tensor(out=XG[:, k, :], in0=X32[:, k, :], scalar=CONST[:, C_G2 + k:C_G2 + k + 1], in1=RSTD[:], op0=ALU.mult, op1=ALU.mult),
                          reads=[("X32", k // 4), "RSTD", "CONST"], writes=[("XG", k)])
                h2ids = [("XG", k) for k in range(16)]
                rhs_h = lambda k: XG[:, k, :]
                si0 = wst_rot.nxt()
                P.dma(WST[si0][:, 0:2, :], skT.rearrange("c p n -> p c n"), writes=[("WST", si0)])
                P.pool(lambda e: e.tensor_copy(out=SKB[:], in_=WST[si0][:, 0:2, :]), reads=[("WST", si0)], writes=["SKB"])
                for n in range(16):
                    ps, pid = linear_chunk(w_qp, 16, n * 128, rhs_h, h2ids)
                    P.act(lambda e, ps=ps, n=n: e.activation(out=QP[:, n, :], in_=ps[:], func=AF.Copy), reads=[pid], writes=[("SQ", n // 4)])

                for j4 in range(4):
                    tsl = slice(j4 * 128, (j4 + 1) * 128)
                    for g in range(4):
                        bi = 3 + (g % 2)
                        for jj in range(4):
                            n = g * 4 + jj
                            P.pe(lambda e, bi=bi, jj=jj, n=n, tsl=tsl: e.matmul(PS[bi][:, jj * 128:(jj + 1) * 128], lhsT=QP[:, n, tsl], rhs=SKB[:, n % 2, :], start=True, stop=True),
                                 reads=[("SQ", n // 4), "SKB"], writes=[("ps", bi)])
                        P.act(lambda e, bi=bi, g=g: e.activation(out=S32[:, 4 * g:4 * g + 4, :], in_=PS[bi][:].rearrange("p (a b) -> p a b", a=4), func=AF.Copy), reads=[("ps", bi)], writes=[("S32", g)])
                    for h in range(8):
                        sid = ("S32", h // 2)
                        for half in range(2):
                            sc = S32[:, 2 * h + half, :]
                            tk = TK[:, half, :]
                            P.dve(lambda e, sc=sc, half=half: e.max(out=V12[:, half, 0:8], in_=sc), reads=[sid], writes=["V12"])
                            P.dve(lambda e, sc=sc, half=half: e.max_index(out=I1[:, half, 0:8], in_max=V12[:, half, 0:8], in_values=sc), reads=[sid, "V12"], writes=["I1"])
                            P.dve(lambda e, sc=sc, tk=tk, half=half: e.match_replace(out=tk, in_to_replace=V12[:, half, 0:8], in_values=sc, imm_value=-1e30), reads=[sid, "V12"], writes=["TK"])
                            P.dve(lambda e, tk=tk, half=half: e.max(out=V12[:, half, 8:16], in_=tk), reads=["TK"], writes=["V12"])
                            P.dve(lambda e, tk=tk, half=half: e.max_index(out=I1[:, half, 8:16], in_max=V12[:, half, 8:16], in_values=tk), reads=["TK", "V12"], writes=["I1"])
                        P.dve(lambda e: e.tensor_copy(out=I1F[:], in_=I1[:]), reads=["I1"], writes=["I1F"])
                        P.dve(lambda e: e.tensor_tensor(out=CAND[:].rearrange("p (a b) -> p a b", a=16), in0=V12[:, 0, :].unsqueeze(2).broadcast_to([128, 16, 16]),
                                                        in1=V12[:, 1, :].unsqueeze(1).broadcast_to([128, 16, 16]), op=ALU.add), reads=["V12"], writes=["CAND"])
                        P.dve(lambda e: e.max(out=C16[:, 0:8], in_=CAND[:]), reads=["CAND"], writes=["C16"])
                        P.dve(lambda e: e.max_index(out=CI[:, 0:8], in_max=C16[:, 0:8], in_values=CAND[:]), reads=["CAND", "C16"], writes=["CI"])
                        P.dve(lambda e: e.match_replace(out=CAND2[:], in_to_replace=C16[:, 0:8], in_values=CAND[:], imm_value=-1e30), reads=["CAND", "C16"], writes=["CAND2"])
                        P.dve(lambda e: e.max(out=C16[:, 8:16], in_=CAND2[:]), reads=["CAND2"], writes=["C16"])
                        P.dve(lambda e: e.max_index(out=CI[:, 8:16], in_max=C16[:, 8:16], in_values=CAND2[:]), reads=["CAND2", "C16"], writes=["CI"])
                        P.dve(lambda e: e.tensor_scalar(out=SM[:, 0:1], in0=C16[:, 0:1], scalar1=-1.0, scalar2=None, op0=ALU.mult), reads=["C16"], writes=["SM0"])
                        P.act(lambda e: e.activation(out=EJ[:], in_=C16[:], func=AF.Exp, bias=SM[:, 0:1], accum_out=SM[:, 1:2]), reads=["C16", "SM0"], writes=["EJ", "SM1"])
                        P.dve(lambda e: e.reciprocal(out=SM[:, 2:3], in_=SM[:, 1:2]), reads=["SM1"], writes=["SM2"])
                        P.dve(lambda e, h=h: e.tensor_scalar(out=ROWC[:, 2, h * 16:(h + 1) * 16], in0=EJ[:], scalar1=SM[:, 2:3], scalar2=None, op0=ALU.mult), reads=["EJ", "SM2"], writes=[("ROWC", 2, h)])
                        P.dve(lambda e: e.tensor_single_scalar(out=CIH[:], in_=CI[:], scalar=4, op=ALU.logical_shift_right), reads=["CI"], writes=["CIH"])
                        P.dve(lambda e: e.tensor_single_scalar(out=CIL[:], in_=CI[:], scalar=15, op=ALU.bitwise_and), reads=["CI"], writes=["CIL"])
                        P.dve(lambda e: e.tensor_copy(out=CIHF[:], in_=CIH[:]), reads=["CIH"], writes=["CIHF"])
                        P.dve(lambda e: e.tensor_copy(out=CILF[:], in_=CIL[:]), reads=["CIL"], writes=["CILF"])
                        for which, (cf, cid) in enumerate(((CIHF, "CIHF"), (CILF, "CILF"))):
                            P.dve(lambda e, cf=cf: e.tensor_tensor(out=OH[:], in0=CONST[:, C_IOTA:C_IOTA + 16].unsqueeze(1).broadcast_to([128, 16, 16]),
                                                                   in1=cf[:].unsqueeze(2).broadcast_to([128, 16, 16]), op=ALU.is_equal), reads=["CONST", cid, "EJ"], writes=["OH"])
                            P.dve(lambda e, which=which: e.tensor_tensor(out=OH[:], in0=OH[:], in1=I1F[:, which, :].unsqueeze(1).broadcast_to([128, 16, 16]), op=ALU.mult), reads=["OH", "I1F"], writes=["OH"])
                            P.dve(lambda e: e.reduce_sum(out=EJ[:], in_=OH[:], axis=mybir.AxisListType.X), reads=["OH"], writes=["EJ"])
                            P.dve(lambda e, which=which, h=h: e.tensor_copy(out=ROWC[:, which, h * 16:(h + 1) * 16], in_=EJ[:]), reads=["EJ"], writes=[("ROWC", which, h)])
                    rc_ids = [("ROWC", w, h) for w in range(3) for h in range(8)]
                    for w in range(3):
                        P.pe(lambda e, w=w: e.matmul(PS[5][:, w * 128:(w + 1) * 128], lhsT=ROWC[:, w, :], rhs=IDB[:], start=True, stop=True), reads=rc_ids + ["IDB"], writes=[("ps", 5)])
                    P.act(lambda e, tsl=tsl: e.activation(out=RTT[:, :, tsl], in_=PS[5][:, 0:384].rearrange("p (a b) -> p a b", a=3), func=AF.Copy), reads=[("ps", 5)], writes=[("RTT", j4)])
                rtt_ids = [("RTT", j) for j in range(4)]
                tap("d_rtt", RTT[:], rtt_ids)
                tap("d_h2", XG[:], h2ids)
                tap("d_qp", QP[:], [("SQ", q) for q in range(4)])

                for qr in range(4):
                    for b8 in range(T // 8):
                        gi = g8_rot.nxt()
                        ts8 = slice(b8 * 8, b8 * 8 + 8)
                        P.dve(lambda e, gi=gi, ts8=ts8: e.tensor_tensor(out=G8[gi][:], in0=CONST[:, C_IOTA:C_IOTA + 128].unsqueeze(1).broadcast_to([128, 8, 128]),
                                                                       in1=RTT[:, 1, ts8].unsqueeze(2).broadcast_to([128, 8, 128]), op=ALU.is_equal), reads=rtt_ids + ["CONST"], writes=[("G8", gi)])
                        P.dve(lambda e, gi=gi, ts8=ts8, qr=qr: e.tensor_tensor(out=R8[gi][:], in0=CONST[:, C_IOTA + 32 * qr:C_IOTA + 32 * qr + 32].unsqueeze(1).broadcast_to([128, 8, 32]),
                                                                              in1=RTT[:, 0, ts8].unsqueeze(2).broadcast_to([128, 8, 32]), op=ALU.is_equal), reads=rtt_ids + ["CONST"], writes=[("R8", gi)])
                        P.pool(lambda e, gi=gi, ts8=ts8: e.tensor_tensor(out=R8[gi][:], in0=R8[gi][:], in1=RTT[:, 2, ts8].unsqueeze(2).broadcast_to([128, 8, 32]), op=ALU.mult), reads=rtt_ids + [("R8", gi)], writes=[("R8", gi)])
                        for j in range(8):
                            P.pe(lambda e, gi=gi, j=j: e.matmul(PS[5][:, j * 32:(j + 1) * 32], lhsT=G8[gi][:, j, :], rhs=R8[gi][:, j, :], start=True, stop=True),
                                 reads=[("G8", gi), ("R8", gi)], writes=[("ps", 5)])
                        P.act(lambda e, ts8=ts8: e.activation(out=WT[:, :, ts8], in_=PS[5][:, 0:256].rearrange("p (t r) -> p r t", t=8), func=AF.Copy), reads=[("ps", 5)], writes=["WT"])
                    if qr == 0:
                        tap("d_wt", WT[:], ["WT"])
                    for g in range(8):
                        grp = []
                        for jj in range(4):
                            rl = g * 4 + jj
                            r = qr * 32 + rl
                            ub, ubid = stream_w(ut, 16, r * 128)
                            si, vi = wst_rot.nxt(), vb_rot.nxt()
                            P.dma(WST[si][:].rearrange("p k n -> p (k n)"), ev[r * 128:(r + 1) * 128, :], writes=[("WST", si)])
                            (P.dve if (rl % 2 == 0) else P.pool)(lambda e, si=si, vi=vi: e.tensor_copy(out=VB[vi][:], in_=WST[si][:].rearrange("p k n -> p (k n)")), reads=[("WST", si)], writes=[("VB", vi)])
                            bi = ps_rot.nxt()
                            for k in range(16):
                                P.pe(lambda e, bi=bi, ub=ub, k=k: e.matmul(PS[bi][:], lhsT=ub[:, k, :], rhs=XG[:, k, :], start=(k == 0), stop=(k == 15)),
                                     reads=[ubid] + h2ids, writes=[("ps", bi)])
                            gi, pi = ga_rot.nxt(), pp_rot.nxt()
                            P.act(lambda e, bi=bi, gi=gi: e.activation(out=GA[gi][:], in_=PS[bi][:], func=AF.Gelu), reads=[("ps", bi)], writes=[("GA", gi)])
                            P.dve(lambda e, gi=gi, pi=pi, rl=rl: e.tensor_tensor(out=PP[pi][:], in0=GA[gi][:], in1=WT[:, rl, :], op=ALU.mult), reads=[("GA", gi), "WT"], writes=[("PP", pi)])
                            grp.append((vi, pi))
                        for n in range(16):
                            bi = 6 + ops_rot.nxt()
                            for jj, (vi, pi) in enumerate(grp):
                                P.pe(lambda e, bi=bi, vi=vi, pi=pi, jj=jj, n=n: e.matmul(PS[bi][:], lhsT=VB[vi][:, n * 128:(n + 1) * 128], rhs=PP[pi][:], start=(jj == 0), stop=(jj == 3)),
                                     reads=[("VB", vi), ("PP", pi)], writes=[("ps", bi)])
                            P.dve(lambda e, bi=bi, n=n: e.tensor_tensor(out=X32[:, n, :], in0=X32[:, n, :], in1=PS[bi][:], op=ALU.add), reads=[("ps", bi), ("X32", n // 4)], writes=[("X32", n // 4)])

                tap("d_xpre", X32[:], [("X32", q) for q in range(4)])
                for q in range(4):
                    P.act(lambda e, q=q: e.activation(out=SQ[:, 4 * q:4 * q + 4, :], in_=X32[:, 4 * q:4 * q + 4, :], func=AF.Square), reads=[("X32", q)], writes=[("SQ", q)])
                rms_from_sq(SQ, 16, [("SQ", q) for q in range(4)], D, RSTD, "RSTD", None, None)
                for k in range(16):
                    P.dve(lambda e, k=k: e.scalar_tensor_tensor(out=X32[:, k, :], in0=X32[:, k, :], scalar=CONST[:, C_GF + k:C_GF + k + 1], in1=RSTD[:], op0=ALU.mult, op1=ALU.mult),
                          reads=[("X32", k // 4), "RSTD", "CONST"], writes=[("X32", k // 4)])
                for q in range(4):
                    st = P.dma(outT[q * 512:(q + 1) * 512, t0:t0 + T].rearrange("(k p) t -> p k t", p=128), X32[:, 4 * q:4 * q + 4, :],
                               reads=[("X32", q)], writes=[("out", s, q)], semkey=("ost", q))
                    final_stores.append(st)
                P.barrier()

    for s_ in range(NSLOT):
        run_slot(s_)
    P.finish(final_waits=final_stores)
    es.close()
    return nc


OFF_CQ, OFF_CKV, OFF_KR, OFF_DQ, OFF_DK, OFF_DV, OFF_GATE = 0, 768, 1280, 1344, 2368, 3392, 4416


def _perm64(cols):
    cols = np.asarray(cols).reshape(-1, 64)
    return np.concatenate([cols[:, 32:], cols[:, :32]], axis=1).reshape(-1)


def _rope_tables(pos):
    inv = (10000.0 ** (-np.arange(0, 64, 2, dtype=np.float32) / np.float32(64))).astype(np.float32)
    ang = (pos.astype(np.float32)[None, :] * inv[:, None]).astype(np.float32)
    cos = np.cos(ang.astype(np.float64)).astype(np.float32)
    sin = np.sin(ang.astype(np.float64)).astype(np.float32)
    cos64 = np.concatenate([cos, cos], 0)
    sin64 = np.concatenate([-sin, sin], 0)
    return np.ascontiguousarray(np.stack([np.concatenate([cos64, cos64], 0), np.concatenate([sin64, sin64], 0)], 0))


_NC_CACHE = {}


def kernel(x, w_in, b_gate, g_norm1, g_cq, w_uq, g_ckv, w_ukv, w_o_mla, lambda_qk, g_subln, w_o_diff,
           w_out, g_norm2, w_q_peer, sub_keys, expert_u, expert_v, g_final):
    f = lambda a: np.asarray(a, dtype=np.float32)
    x = f(x)
    w_in0 = f(w_in)[0]
    ar = np.arange
    ckv = ar(OFF_CKV, OFF_KR)
    kr = ar(OFF_KR, OFF_DQ)
    kv_cols = [ckv, kr, kr, _perm64(kr), _perm64(kr)]
    for h in range(8):
        dk = ar(OFF_DK + h * 128, OFF_DK + (h + 1) * 128)
        kv_cols += [dk, _perm64(dk)]
    W_kv = np.ascontiguousarray(w_in0[:, np.concatenate(kv_cols)])
    W_dv = np.ascontiguousarray(w_in0[:, OFF_DV:OFF_GATE])
    q_cols = [ar(OFF_CQ, OFF_CKV)]
    for h in range(8):
        dq = ar(OFF_DQ + h * 128, OFF_DQ + (h + 1) * 128)
        q_cols += [dq, _perm64(dq)]
    W_q = np.ascontiguousarray(w_in0[:, np.concatenate(q_cols)])
    W_gate = np.ascontiguousarray(w_in0[:, OFF_GATE:])
    wuq = f(w_uq)[0]
    nope = np.concatenate([ar(h * 192, h * 192 + 128) for h in range(8)])
    rope = np.concatenate([ar(h * 192 + 128, h * 192 + 192) for h in range(8)])
    W_uq2 = np.ascontiguousarray(wuq[:, np.concatenate([nope, rope, _perm64(rope)])])
    wukv = f(w_ukv)[0]
    W_uk = np.ascontiguousarray(wukv[:, np.concatenate([ar(h * 256, h * 256 + 128) for h in range(8)])])
    W_uv = np.ascontiguousarray(wukv[:, np.concatenate([ar(h * 256 + 128, h * 256 + 256) for h in range(8)])])
    skT = np.ascontiguousarray(f(sub_keys)[0].transpose(0, 2, 1))
    ut = np.ascontiguousarray(f(expert_u)[0].T)
    ev = np.ascontiguousarray(f(expert_v)[0])
    col = lambda v: np.ascontiguousarray(f(v).reshape(-1, 128).T)
    consts = np.zeros((128, NC_CONST), np.float32)
    consts[:, C_G1:C_G1 + 16] = col(g_norm1[0])
    consts[:, C_GCQ:C_GCQ + 6] = col(g_cq[0])
    consts[:, C_GCKV:C_GCKV + 4] = col(g_ckv[0])
    consts[:, C_BG:C_BG + 32] = col(b_gate[0])
    consts[:, C_GSUB:C_GSUB + 1] = col(g_subln[0])
    consts[:, C_G2:C_G2 + 16] = col(g_norm2[0])
    consts[:, C_GF:C_GF + 16] = col(g_final)
    consts[:, C_LAM:C_LAM + 256] = f(lambda_qk)[0].reshape(1, 256)
    consts[:, C_KPOS:C_KPOS + 64] = (ar(64)[None, :] * 128 + ar(128)[:, None]).astype(np.float32)
    consts[:, C_IOTA:C_IOTA + 128] = ar(128, dtype=np.float32)[None, :]
    consts[:, C_IDENT:C_IDENT + 128] = np.eye(128, dtype=np.float32)
    cs_seq = _rope_tables(ar(SEQ))
    shared = {"consts": consts, "cs_seq": cs_seq, "w_kv": W_kv, "w_dv": W_dv, "w_q": W_q, "w_gate": W_gate,
              "w_uq2": W_uq2, "w_uk": W_uk, "w_uv": W_uv, "w_oa": np.ascontiguousarray(f(w_o_mla)[0]),
              "w_ob": np.ascontiguousarray(f(w_o_diff)[0]), "w_out": np.ascontiguousarray(f(w_out)[0]),
              "w_qp": np.ascontiguousarray(f(w_q_peer)[0]), "skT": skT, "ut": ut, "ev": ev}
    in_maps, own_pos = [], []
    xT = [np.ascontiguousarray(x[b].T) for b in range(2)]
    for c in range(8):
        b, p = c // 4, c % 4
        groups = [p, 7 - p, 8 + p, 15 - p]
        pos = np.concatenate([ar(g * 512, (g + 1) * 512) for g in groups])
        own_pos.append((b, pos))
        m = dict(shared)
        m["xT_seq"] = xT[b]
        m["xT_own"] = np.ascontiguousarray(xT[b][:, pos])
        m["cs_own"] = _rope_tables(pos)
        m["qpos"] = np.ascontiguousarray(np.broadcast_to(pos.astype(np.float32)[None, :], (128, 2048)))
        in_maps.append(m)
    if "nc" not in _NC_CACHE:
        _NC_CACHE["nc"] = build_program()
    res = run_bass_kernel_spmd(_NC_CACHE["nc"], in_maps, core_ids=list(range(8)))
    if DEBUG:
        _DBG["res"] = res.results
    out = np.zeros((2, SEQ, D), np.float32)
    for c in range(8):
        b, pos = own_pos[c]
        out[b, pos, :] = res.results[c]["outT"].T
    return out
```

```python
import math
from contextlib import ExitStack
import numpy as np
import concourse.bass as bass
import concourse.mybir as mybir
from concourse.bass_utils import run_bass_kernel_spmd

F32 = mybir.dt.float32
BF16 = mybir.dt.bfloat16
ALU = mybir.AluOpType
AF = mybir.ActivationFunctionType
ENGS = ("pe", "act", "dve", "pool", "sp")

D = 2048
SEQ = 8192
T = 512
EPS = 1e-6
NSLOT = 4
NE = 16384
DEBUG = False
_DBG = {}


class _Ins:
    __slots__ = ("eng", "fn", "deps", "dma", "semkey", "val", "signal", "idx")

    def __init__(self, eng, fn, dma, semkey):
        self.idx = 0
        self.eng = eng
        self.fn = fn
        self.dma = dma
        self.semkey = semkey
        self.deps = set()
        self.val = None
        self.signal = False


class Prog:
    def __init__(self, nc):
        self.nc = nc
        self.ins = []
        self.last_write = {}
        self.readers = {}
        self.dma_counts = {}
        self.dma_last = {}
        self.eng_last = {}
        self.epoch = set()

    def op(self, eng, fn, reads=(), writes=(), dma=False, semkey=None):
        if dma and semkey is None:
            semkey = ("auto", writes[0])
        i = _Ins(eng, fn, dma, semkey)
        deps = set(self.epoch)
        for b in reads:
            w = self.last_write.get(b)
            if w is not None:
                deps.add(w)
        for b in writes:
            w = self.last_write.get(b)
            if w is not None:
                deps.add(w)
            for r in self.readers.get(b, ()):
                deps.add(r)
        for b in reads:
            self.readers.setdefault(b, []).append(i)
        for b in writes:
            self.last_write[b] = i
            self.readers[b] = []
        if dma:
            c = self.dma_counts.get(semkey, 0) + 16
            self.dma_counts[semkey] = c
            i.val = c
            self.dma_last[semkey] = i
        else:
            self.eng_last[eng] = i
        i.deps = deps
        i.idx = len(self.ins)
        self.ins.append(i)
        return i

    def barrier(self):
        self.epoch = set(self.eng_last.values()) | set(self.dma_last.values())
        self.last_write = {}
        self.readers = {}

    def pe(self, fn, reads=(), writes=()):
        return self.op("pe", fn, reads, writes)

    def act(self, fn, reads=(), writes=()):
        return self.op("act", fn, reads, writes)

    def dve(self, fn, reads=(), writes=()):
        return self.op("dve", fn, reads, writes)

    def pool(self, fn, reads=(), writes=()):
        return self.op("pool", fn, reads, writes)

    def dma(self, out, in_, reads=(), writes=(), semkey=None):
        return self.op("sp", lambda e: e.dma_start(out=out, in_=in_), reads, writes,
                       dma=True, semkey=semkey)

    def finish(self, final_waits=()):
        nc = self.nc
        for i in self.ins:
            if i.eng == "pe" and not i.dma:
                i.deps = {d for d in i.deps if not (d.eng == "pe" and not d.dma)}
        for i in self.ins:
            best = {}
            for d in i.deps:
                key = ("d", d.semkey) if d.dma else ("e", d.eng)
                if key not in best or best[key].idx < d.idx:
                    best[key] = d
            i.deps = set(best.values())
        for i in self.ins:
            for d in i.deps:
                d.signal = True
        for d in final_waits:
            d.signal = True
        cnt = {e: 0 for e in ENGS}
        for i in self.ins:
            if not i.dma and i.signal:
                cnt[i.eng] += 1
                i.val = cnt[i.eng]
        with ExitStack() as es:
            esem = {e: es.enter_context(nc.semaphore("s_" + e)) for e in ENGS}
            dsem = {}
            for k in self.dma_counts:
                dsem[k] = es.enter_context(nc.semaphore("d%d" % len(dsem)))

            def tok(d):
                return (dsem[d.semkey] if d.dma else esem[d.eng]), d.val

            per = {e: [] for e in ENGS}
            for i in self.ins:
                per[i.eng].append(i)
            block = es.enter_context(nc.Block())

            def emit(engname, eh):
                waited = {}
                for i in per[engname]:
                    ws = {}
                    for d in i.deps:
                        s, v = tok(d)
                        key = id(s)
                        if waited.get(key, 0) >= v:
                            continue
                        if key not in ws or ws[key][1] < v:
                            ws[key] = (s, v)
                    for key, (s, v) in ws.items():
                        eh.wait_ge(s, v)
                        waited[key] = v
                    r = i.fn(eh)
                    if i.dma:
                        r.then_inc(dsem[i.semkey], 16)
                    elif i.signal:
                        r.then_inc(esem[i.eng], 1)
                if engname == "sp":
                    for d in final_waits:
                        s, v = tok(d)
                        eh.wait_ge(s, v)

            @block.tensor
            def _(e):
                emit("pe", e)

            @block.scalar
            def _(e):
                emit("act", e)

            @block.vector
            def _(e):
                emit("dve", e)

            @block.gpsimd
            def _(e):
                emit("pool", e)

            @block.sync
            def _(e):
                emit("sp", e)
        return cnt


class Rot:
    def __init__(self, n):
        self.n = n
        self.i = 0

    def nxt(self):
        r = self.i
        self.i = (self.i + 1) % self.n
        return r


C_G1, C_GCQ, C_GCKV, C_BG, C_GSUB, C_G2, C_GF = 0, 16, 22, 26, 58, 59, 75
C_LAM = 91
C_KPOS = 347
C_IOTA = 411
C_IDENT = 539
NC_CONST = 667

NKV = 22
NQ = 22


def build_program():
    nc = bass.Bass("TRN2", target_bir_lowering=False)
    dt_in = lambda n, s: nc.dram_tensor(n, s, F32, kind="ExternalInput").ap()
    xT_seq = dt_in("xT_seq", [D, SEQ])
    xT_own = dt_in("xT_own", [D, 2048])
    cs_seq = dt_in("cs_seq", [2, 128, SEQ])
    cs_own = dt_in("cs_own", [2, 128, 2048])
    qpos_d = dt_in("qpos", [128, 2048])
    consts_d = dt_in("consts", [128, NC_CONST])
    w_kv = dt_in("w_kv", [D, NKV * 128])
    w_dv = dt_in("w_dv", [D, 1024])
    w_q = dt_in("w_q", [D, NQ * 128])
    w_gate = dt_in("w_gate", [D, 4096])
    w_uq2 = dt_in("w_uq2", [768, 2048])
    w_uk = dt_in("w_uk", [512, 1024])
    w_uv = dt_in("w_uv", [512, 1024])
    w_oa = dt_in("w_oa", [1024, D])
    w_ob = dt_in("w_ob", [1024, D])
    w_out = dt_in("w_out", [D, D])
    w_qp = dt_in("w_qp", [D, D])
    skT = dt_in("skT", [2, 128, 128])
    ut = dt_in("ut", [D, NE])
    ev = dt_in("ev", [NE, D])
    outT = nc.dram_tensor("outT", [D, 2048], F32, kind="ExternalOutput").ap()
    skind = "ExternalOutput" if DEBUG else "Internal"
    kTm = nc.dram_tensor("kTm", [8, 128, SEQ], BF16, kind=skind).ap()
    kTr = nc.dram_tensor("kTr", [128, SEQ], BF16, kind=skind).ap()
    kTd = nc.dram_tensor("kTd", [8, 128, SEQ], BF16, kind=skind).ap()
    vM = nc.dram_tensor("vM", [SEQ, 1024], BF16, kind=skind).ap()
    vD = nc.dram_tensor("vD", [SEQ, 1024], BF16, kind=skind).ap()
    taps = {}
    if DEBUG:
        for nm, shp, dtp in (("d_x1", [128, 16, T], F32), ("d_otm", [128, 8, T], BF16), ("d_otd", [128, 8, T], BF16),
                             ("d_qtm", [128, 8, T], BF16), ("d_qtr", [128, 4, T], BF16), ("d_qtd", [128, 8, T], BF16),
                             ("d_h2", [128, 16, T], BF16), ("d_rtt", [128, 3, T], F32), ("d_qp", [128, 16, T], BF16),
                             ("d_xpre", [128, 16, T], F32), ("d_wt", [128, 32, T], BF16)):
            taps[nm] = nc.dram_tensor(nm, shp, dtp, kind="ExternalOutput").ap()

    def tap(nm, tile_ap, ids):
        if DEBUG and nm in taps:
            final_stores.append(P.dma(taps.pop(nm), tile_ap, reads=ids, writes=[("tap", nm)], semkey=("tap", nm)))


    final_stores = []
    es = ExitStack()
    sb = lambda n, s, d: es.enter_context(nc.sbuf_tensor(n, s, d))
    PS = [es.enter_context(nc.psum_tensor("ps%d" % i, [128, 512], F32)) for i in range(8)]
    P = Prog(nc)

    CONST = sb("CONST", [128, NC_CONST], F32)
    ONES = sb("ONES", [128, 128], BF16)
    IDB = sb("IDB", [128, 128], BF16)
    SMALL = sb("SMALL", [128, 16], F32)
    LAMT = sb("LAMT", [128, 128], F32)
    P.dma(CONST[:], consts_d, writes=["CONST"])
    P.pool(lambda e: e.memset(ONES[:], 1.0), writes=["ONES"])
    P.dve(lambda e: e.tensor_copy(out=IDB[:], in_=CONST[:, C_IDENT:C_IDENT + 128]), reads=["CONST"], writes=["IDB"])
    lam_init = 0.8 - 0.6 * math.exp(-0.3 * 0)
    P.dve(lambda e: e.tensor_tensor(out=LAMT[:, 0:64], in0=CONST[:, C_LAM:C_LAM + 64], in1=CONST[:, C_LAM + 64:C_LAM + 128], op=ALU.mult), reads=["CONST"], writes=["LAMT0"])
    P.dve(lambda e: e.tensor_tensor(out=LAMT[:, 64:128], in0=CONST[:, C_LAM + 128:C_LAM + 192], in1=CONST[:, C_LAM + 192:C_LAM + 256], op=ALU.mult), reads=["CONST"], writes=["LAMT1"])
    P.dve(lambda e: e.reduce_sum(out=SMALL[:, 2:3], in_=LAMT[:, 0:64], axis=mybir.AxisListType.X), reads=["LAMT0"], writes=["SM2"])
    P.dve(lambda e: e.reduce_sum(out=SMALL[:, 3:4], in_=LAMT[:, 64:128], axis=mybir.AxisListType.X), reads=["LAMT1"], writes=["SM3"])
    P.act(lambda e: e.activation(out=SMALL[:, 4:6], in_=SMALL[:, 2:4], func=AF.Exp), reads=["SM2", "SM3"], writes=["SM45"])
    P.dve(lambda e: e.tensor_tensor(out=SMALL[:, 6:7], in0=SMALL[:, 5:6], in1=SMALL[:, 4:5], op=ALU.subtract), reads=["SM45"], writes=["SM6"])
    P.dve(lambda e: e.tensor_scalar(out=SMALL[:, 0:1], in0=SMALL[:, 6:7], scalar1=-lam_init, scalar2=None, op0=ALU.add), reads=["SM6"], writes=["NEGLAM"])
    P.dve(lambda e: e.tensor_scalar(out=SMALL[:, 1:2], in0=CONST[:, C_GSUB:C_GSUB + 1], scalar1=1.0 - lam_init, scalar2=None, op0=ALU.mult), reads=["CONST"], writes=["GSUBS"])

    XG = sb("XG", [128, 16, T], BF16)
    RSTD = sb("RSTD", [128, T], F32)
    RSTDT = sb("RSTDT", [128, 8], F32)
    TMPS = sb("TMPS", [128, T], F32)
    WST = [sb("WST%d" % i, [128, 16, 128], F32) for i in range(3)]
    WB = [sb("WB%d" % i, [128, 16, 128], BF16) for i in range(3)]
    wst_rot, wb_rot, ps_rot, cast_rot = Rot(3), Rot(3), Rot(2), Rot(3)

    def stream_w(src, kc, col0, ncols=128):
        si, bi = wst_rot.nxt(), wb_rot.nxt()
        st, wb = WST[si], WB[bi]
        P.dma(st[:, 0:kc, 0:ncols], src[0:kc * 128, col0:col0 + ncols].rearrange("(k p) n -> p k n", p=128),
              writes=[("WST", si)])
        if cast_rot.nxt() == 2:
            P.dve(lambda e: e.tensor_copy(out=wb[:, 0:kc, 0:ncols], in_=st[:, 0:kc, 0:ncols]), reads=[("WST", si)], writes=[("WB", bi)])
        else:
            P.act(lambda e: e.activation(out=wb[:, 0:kc, 0:ncols], in_=st[:, 0:kc, 0:ncols], func=AF.Copy), reads=[("WST", si)], writes=[("WB", bi)])
        return wb, ("WB", bi)

    def linear_chunk(src, kc, col0, rhs_fn, rhs_ids, pbank=None):
        wb, wid = stream_w(src, kc, col0)
        bi = ps_rot.nxt() if pbank is None else pbank
        ps = PS[bi]
        for k in range(kc):
            P.pe(lambda e, k=k: e.matmul(ps[:], lhsT=wb[:, k, :], rhs=rhs_fn(k), start=(k == 0), stop=(k == kc - 1)),
                 reads=[wid] + rhs_ids, writes=[("ps", bi)])
        return ps, ("ps", bi)

    def load_x_and_norm(X32, SQ, src, t0, gcol, tokmajor):
        for q in range(4):
            P.dma(X32[:, 4 * q:4 * q + 4, :], src[q * 512:(q + 1) * 512, t0:t0 + T].rearrange("(k p) t -> p k t", p=128),
                  writes=[("X32", q)])
        x32ids = [("X32", q) for q in range(4)]
        for q in range(4):
            P.act(lambda e, q=q: e.activation(out=SQ[:, 4 * q:4 * q + 4, :], in_=X32[:, 4 * q:4 * q + 4, :], func=AF.Square),
                  reads=[("X32", q)], writes=[("SQ", q)])
        sqids = [("SQ", q) for q in range(4)]
        rms_from_sq(SQ, 16, sqids, D, RSTD, "RSTD", RSTDT if tokmajor else None, "RSTDT")
        for k in range(16):
            P.act(lambda e, k=k: e.activation(out=XG[:, k, :], in_=X32[:, k, :], func=AF.Copy, scale=CONST[:, gcol + k:gcol + k + 1]),
                  reads=[("X32", k // 4), "CONST"], writes=[("XG", k)])
        return x32ids, [("XG", k) for k in range(16)]

    def rms_from_sq(sq, kc, sqids, dim, rstd, rid, rstdt, rtid):
        ps = PS[2]
        for k in range(kc):
            P.pe(lambda e, k=k: e.matmul(ps[:], lhsT=ONES[:], rhs=sq[:, k, :], start=(k == 0), stop=(k == kc - 1)),
                 reads=["ONES"] + sqids, writes=[("ps", 2)])
        P.act(lambda e: e.activation(out=rstd[:], in_=ps[:], func=AF.Sqrt, scale=1.0 / dim, bias=EPS), reads=[("ps", 2)], writes=[rid + "s"])
        P.dve(lambda e: e.reciprocal(out=rstd[:], in_=rstd[:]), reads=[rid + "s"], writes=[rid])
        if rstdt is not None:
            ps3 = PS[3]
            for j in range(4):
                for k in range(kc):
                    P.pe(lambda e, k=k, j=j: e.matmul(ps3[:, j:j + 1], lhsT=sq[:, k, j * 128:(j + 1) * 128], rhs=ONES[:, 0:1], start=(k == 0), stop=(k == kc - 1)),
                         reads=["ONES"] + sqids, writes=[("ps", 3)])
            P.act(lambda e: e.activation(out=rstdt[:, 0:4], in_=ps3[:, 0:4], func=AF.Sqrt, scale=1.0 / dim, bias=EPS), reads=[("ps", 3)], writes=[rtid + "s"])
            P.dve(lambda e: e.reciprocal(out=rstdt[:, 0:4], in_=rstdt[:, 0:4]), reads=[rtid + "s"], writes=[rtid])

    def run_phase1():
        with ExitStack() as es1:
            sb1 = lambda n, s, d: es1.enter_context(nc.sbuf_tensor(n, s, d))
            X32 = sb1("X32p1", [128, 16, T], F32)
            SQ = sb1("SQp1", [128, 16, T], BF16)
            WDV = sb1("WDV", [128, 16, 1024], BF16)
            WUK = sb1("WUK", [128, 4, 1024], BF16)
            WUV = sb1("WUV", [128, 4, 1024], BF16)
            CKV32 = sb1("CKV32", [128, 4, T], F32)
            SQ2 = sb1("SQ2", [128, 4, T], BF16)
            RSTD2 = sb1("RSTD2", [128, T], F32)
            CKVN = sb1("CKVN", [128, 4, T], BF16)
            CSK = sb1("CSK", [128, 2, T], F32)
            RA = sb1("RA", [128, T], F32)
            RB = sb1("RB", [128, T], F32)
            KOUT = [sb1("KOUT%d" % i, [128, T], BF16) for i in range(2)]
            VOUT = [sb1("VOUT%d" % i, [128, 1024], BF16) for i in range(2)]
            ko_rot, vo_rot, vps_rot = Rot(2), Rot(2), Rot(2)
            for c8 in range(8):
                wb, wid = stream_w(w_dv, 16, c8 * 128)
                P.pool(lambda e, wb=wb, c8=c8: e.tensor_copy(out=WDV[:, :, c8 * 128:(c8 + 1) * 128], in_=wb[:, :, :]), reads=[wid], writes=[("WDV", c8)])
                wb, wid = stream_w(w_uk, 4, c8 * 128)
                P.pool(lambda e, wb=wb, c8=c8: e.tensor_copy(out=WUK[:, :, c8 * 128:(c8 + 1) * 128], in_=wb[:, 0:4, :]), reads=[wid], writes=[("WUK", c8)])
                wb, wid = stream_w(w_uv, 4, c8 * 128)
                P.pool(lambda e, wb=wb, c8=c8: e.tensor_copy(out=WUV[:, :, c8 * 128:(c8 + 1) * 128], in_=wb[:, 0:4, :]), reads=[wid], writes=[("WUV", c8)])
            wdv_ids = [("WDV", c) for c in range(8)]
            wuk_ids = [("WUK", c) for c in range(8)]
            wuv_ids = [("WUV", c) for c in range(8)]

            def rope_store(psA_fn, psB_fn, dst):
                psA, idA = psA_fn()
                P.dve(lambda e: e.tensor_tensor(out=RA[:], in0=psA[:], in1=CSK[:, 0, :], op=ALU.mult), reads=[idA, "CSKR"], writes=["RA"])
                psB, idB = psB_fn()
                P.dve(lambda e: e.tensor_tensor(out=RB[:], in0=psB[:], in1=CSK[:, 1, :], op=ALU.mult), reads=[idB, "CSKR"], writes=["RB"])
                ki = ko_rot.nxt()
                P.pool(lambda e: e.tensor_tensor(out=KOUT[ki][:], in0=RA[:], in1=RB[:], op=ALU.add), reads=["RA", "RB"], writes=[("KOUT", ki)])
                P.dma(dst, KOUT[ki][:], reads=[("KOUT", ki)], writes=[("kscr", ki)], semkey=("kst", ki))

            for it in range(SEQ // T):
                t0 = it * T
                x32ids, xgids = load_x_and_norm(X32, SQ, xT_seq, t0, C_G1, True)
                P.dma(CSK[:], cs_seq[:, :, t0:t0 + T].rearrange("c p t -> p c t"), writes=["CSK", "CSKR"])
                for c2 in range(2):
                    P.dve(lambda e, c2=c2: e.tensor_tensor(out=CSK[:, c2, :], in0=CSK[:, c2, :], in1=RSTD[:], op=ALU.mult), reads=["CSK", "RSTD"], writes=["CSKR"])
                rhs_x = lambda k: XG[:, k, :]
                for n in range(4):
                    ps, pid = linear_chunk(w_kv, 16, n * 128, rhs_x, xgids)
                    P.dve(lambda e, ps=ps, n=n: e.tensor_tensor(out=CKV32[:, n, :], in0=ps[:], in1=RSTD[:], op=ALU.mult), reads=[pid, "RSTD"], writes=[("CKV32", n)])
                    P.act(lambda e, n=n: e.activation(out=SQ2[:, n, :], in_=CKV32[:, n, :], func=AF.Square), reads=[("CKV32", n)], writes=[("SQ2", n)])
                rope_store(lambda: linear_chunk(w_kv, 16, 4 * 128, rhs_x, xgids), lambda: linear_chunk(w_kv, 16, 5 * 128, rhs_x, xgids), kTr[:, t0:t0 + T])
                for h in range(8):
                    rope_store(lambda h=h: linear_chunk(w_kv, 16, (6 + 2 * h) * 128, rhs_x, xgids),
                               lambda h=h: linear_chunk(w_kv, 16, (7 + 2 * h) * 128, rhs_x, xgids), kTd[h, :, t0:t0 + T])
                sq2ids = [("SQ2", n) for n in range(4)]
                rms_from_sq(SQ2, 4, sq2ids, 512, RSTD2, "RSTD2", None, None)
                for n in range(4):
                    P.dve(lambda e, n=n: e.scalar_tensor_tensor(out=CKVN[:, n, :], in0=CKV32[:, n, :], scalar=CONST[:, C_GCKV + n:C_GCKV + n + 1], in1=RSTD2[:], op0=ALU.mult, op1=ALU.mult),
                          reads=[("CKV32", n), "RSTD2", "CONST"], writes=[("CKVN", n)])
                ckvn_ids = [("CKVN", n) for n in range(4)]
                for h in range(8):
                    bi = ps_rot.nxt()
                    ps = PS[bi]
                    for j in range(4):
                        P.pe(lambda e, ps=ps, j=j, h=h: e.matmul(ps[:], lhsT=WUK[:, j, h * 128:(h + 1) * 128], rhs=CKVN[:, j, :], start=(j == 0), stop=(j == 3)),
                             reads=wuk_ids + ckvn_ids, writes=[("ps", bi)])
                    ki = ko_rot.nxt()
                    P.act(lambda e, ps=ps, ki=ki: e.activation(out=KOUT[ki][:], in_=ps[:], func=AF.Copy), reads=[("ps", bi)], writes=[("KOUT", ki)])
                    P.dma(kTm[h, :, t0:t0 + T], KOUT[ki][:], reads=[("KOUT", ki)], writes=[("kscr", ki)], semkey=("kst", ki))
                for j4 in range(4):
                    vi = vo_rot.nxt()
                    for half in range(2):
                        bi = 4 + vps_rot.nxt()
                        ps = PS[bi]
                        for j in range(4):
                            P.pe(lambda e, ps=ps, j=j, j4=j4, half=half: e.matmul(ps[:], lhsT=CKVN[:, j, j4 * 128:(j4 + 1) * 128], rhs=WUV[:, j, half * 512:(half + 1) * 512], start=(j == 0), stop=(j == 3)),
                                 reads=wuv_ids + ckvn_ids, writes=[("ps", bi)])
                        P.act(lambda e, ps=ps, vi=vi, half=half: e.activation(out=VOUT[vi][:, half * 512:(half + 1) * 512], in_=ps[:], func=AF.Copy), reads=[("ps", bi)], writes=[("VOUT", vi, half)])
                    P.dma(vM[t0 + j4 * 128:t0 + (j4 + 1) * 128, :], VOUT[vi][:], reads=[("VOUT", vi, 0), ("VOUT", vi, 1)], writes=[("vscr", vi)], semkey=("vst", vi))
                    vi = vo_rot.nxt()
                    for half in range(2):
                        bi = 4 + vps_rot.nxt()
                        ps = PS[bi]
                        for k in range(16):
                            P.pe(lambda e, ps=ps, k=k, j4=j4, half=half: e.matmul(ps[:], lhsT=XG[:, k, j4 * 128:(j4 + 1) * 128], rhs=WDV[:, k, half * 512:(half + 1) * 512], start=(k == 0), stop=(k == 15)),
                                 reads=wdv_ids + xgids, writes=[("ps", bi)])
                        P.act(lambda e, ps=ps, vi=vi, half=half, j4=j4: e.activation(out=VOUT[vi][:, half * 512:(half + 1) * 512], in_=ps[:], func=AF.Copy, scale=RSTDT[:, j4:j4 + 1]),
                              reads=[("ps", bi), "RSTDT"], writes=[("VOUT", vi, half)])
                    P.dma(vD[t0 + j4 * 128:t0 + (j4 + 1) * 128, :], VOUT[vi][:], reads=[("VOUT", vi, 0), ("VOUT", vi, 1)], writes=[("vscr", vi)], semkey=("vst", vi))
            P.barrier()


    run_phase1()

    def run_slot(s):
        t0 = s * T
        nkt = 16 * (s + 1)
        with ExitStack() as esS:
            sbS = lambda n, sh, d: esS.enter_context(nc.sbuf_tensor("%s_%d" % (n, s), sh, d))
            X32 = sbS("X32", [128, 16, T], F32)
            SQ = sbS("SQ", [128, 16, T], BF16)
            with ExitStack() as esO:
                sbO = lambda n, sh, d: esO.enter_context(nc.sbuf_tensor("%s_%d" % (n, s), sh, d))
                OTM = sbO("OTM", [128, 8, T], BF16)
                OTD = sbO("OTD", [128, 8, T], BF16)
                with ExitStack() as esQ:
                    sbQ = lambda n, sh, d: esQ.enter_context(nc.sbuf_tensor("%s_%d" % (n, s), sh, d))
                    QTM = sbQ("QTM", [128, 8, T], BF16)
                    QTR = sbQ("QTR", [128, 4, T], BF16)
                    QTD = sbQ("QTD", [128, 8, T], BF16)
                    with ExitStack() as esA:
                        sbA = lambda n, sh, d: esA.enter_context(nc.sbuf_tensor("%s_%d" % (n, s), sh, d))
                        CQ32 = sbA("CQ32", [128, 6, T], F32)
                        SQ3 = sbA("SQ3", [128, 6, T], BF16)
                        CQN = sbA("CQN", [128, 6, T], BF16)
                        CSQ = sbA("CSQ", [128, 2, T], F32)
                        RA = sbA("RA2", [128, T], F32)
                        RB = sbA("RB2", [128, T], F32)
                        x32ids, xgids = load_x_and_norm(X32, SQ, xT_own, t0, C_G1, False)
                        P.dma(CSQ[:], cs_own[:, :, t0:t0 + T].rearrange("c p t -> p c t"), writes=["CSQ"])
                        rhs_x = lambda k: XG[:, k, :]

                        CSQR = sbA("CSQR", [128, 2, T], F32)
                        for c2 in range(2):
                            P.dve(lambda e, c2=c2: e.tensor_tensor(out=CSQR[:, c2, :], in0=CSQ[:, c2, :], in1=RSTD[:], op=ALU.mult), reads=["CSQ", "RSTD"], writes=[("CSQR", c2)])

                        def rope_q(psA_fn, psB_fn, dst, dst_id, scaled):
                            tab = CSQR if scaled else CSQ
                            tids = [("CSQR", 0), ("CSQR", 1)] if scaled else ["CSQ"]
                            psA, idA = psA_fn()
                            P.dve(lambda e: e.tensor_tensor(out=RA[:], in0=psA[:], in1=tab[:, 0, :], op=ALU.mult), reads=[idA] + tids, writes=["RA"])
                            psB, idB = psB_fn()
                            P.dve(lambda e: e.tensor_tensor(out=RB[:], in0=psB[:], in1=tab[:, 1, :], op=ALU.mult), reads=[idB] + tids, writes=["RB"])
                            P.pool(lambda e: e.tensor_tensor(out=dst, in0=RA[:], in1=RB[:], op=ALU.add), reads=["RA", "RB"], writes=[dst_id])

                        for n in range(6):
                            ps, pid = linear_chunk(w_q, 16, n * 128, rhs_x, xgids)
                            P.dve(lambda e, ps=ps, n=n: e.tensor_tensor(out=CQ32[:, n, :], in0=ps[:], in1=RSTD[:], op=ALU.mult), reads=[pid, "RSTD"], writes=[("CQ32", n)])
                            P.act(lambda e, n=n: e.activation(out=SQ3[:, n, :], in_=CQ32[:, n, :], func=AF.Square), reads=[("CQ32", n)], writes=[("SQ3", n)])
                        for h in range(8):
                            rope_q(lambda h=h: linear_chunk(w_q, 16, (6 + 2 * h) * 128, rhs_x, xgids),
                                   lambda h=h: linear_chunk(w_q, 16, (7 + 2 * h) * 128, rhs_x, xgids), QTD[:, h, :], ("QTD", h), True)
                        sq3ids = [("SQ3", n) for n in range(6)]
                        rms_from_sq(SQ3, 6, sq3ids, 768, TMPS, "TMPS", None, None)
                        for n in range(6):
                            P.dve(lambda e, n=n: e.scalar_tensor_tensor(out=CQN[:, n, :], in0=CQ32[:, n, :], scalar=CONST[:, C_GCQ + n:C_GCQ + n + 1], in1=TMPS[:], op0=ALU.mult, op1=ALU.mult),
                                  reads=[("CQ32", n), "TMPS", "CONST"], writes=[("CQN", n)])
                        cqn_ids = [("CQN", n) for n in range(6)]
                        rhs_c = lambda k: CQN[:, k, :]
                        for h in range(8):
                            ps, pid = linear_chunk(w_uq2, 6, h * 128, rhs_c, cqn_ids)
                            P.act(lambda e, ps=ps, h=h: e.activation(out=QTM[:, h, :], in_=ps[:], func=AF.Copy), reads=[pid], writes=[("QTM", h)])
                        for j in range(4):
                            rope_q(lambda j=j: linear_chunk(w_uq2, 6, (8 + j) * 128, rhs_c, cqn_ids),
                                   lambda j=j: linear_chunk(w_uq2, 6, (12 + j) * 128, rhs_c, cqn_ids), QTR[:, j, :], ("QTR", j), False)
                        tap("d_qtm", QTM[:], [("QTM", h) for h in range(8)])
                        tap("d_qtr", QTR[:], [("QTR", h) for h in range(4)])
                        tap("d_qtd", QTD[:], [("QTD", h) for h in range(8)])
                        P.barrier()
                    with ExitStack() as esB:
                        sbB = lambda n, sh, d: esB.enter_context(nc.sbuf_tensor("%s_%d" % (n, s), sh, d))
                        QPOS = sbB("QPOS", [128, T], F32)
                        KC = [sbB("KC%d" % i, [128, 2048], BF16) for i in range(3)]
                        KRC = [sbB("KRC%d" % i, [128, 2048], BF16) for i in range(3)]
                        VC = [sbB("VC%d" % i, [128, 16, 128], BF16) for i in range(3)]
                        PT = [sbB("PT%d" % i, [128, T], BF16) for i in range(3)]
                        A1 = sbB("A1", [128, T], F32)
                        A2 = sbB("A2", [128, T], F32)
                        RL = sbB("RL", [128, T], F32)
                        SQD = sbB("SQD", [128, 1, T], BF16)
                        kc_rot, pt_rot = Rot(3), Rot(3)
                        P.dma(QPOS[:], qpos_d[:, t0:t0 + T], writes=["QPOS"])

                        def attend(ksrc, vsrc, h, with_rope, streams):
                            state = {"ci": None, "kids": None}

                            def qk(kt, stream):
                                c16, kk = kt // 16, kt % 16
                                if kk == 0 and stream is streams[0]:
                                    ci = kc_rot.nxt()
                                    ks = slice(c16 * 2048, (c16 + 1) * 2048)
                                    P.dma(KC[ci][:], ksrc[h, :, ks], writes=[("KC", ci)])
                                    P.dma(VC[ci][:], vsrc[ks, h * 128:(h + 1) * 128].rearrange("(kt p) d -> p kt d", p=128), writes=[("VC", ci)])
                                    kids = [("KC", ci)]
                                    if with_rope:
                                        P.dma(KRC[ci][:], kTr[:, ks], writes=[("KRC", ci)])
                                        kids.append(("KRC", ci))
                                    state["ci"], state["kids"] = ci, kids
                                ci, kids = state["ci"], state["kids"]
                                (parts_fn, q_ids, scale, o_bank, l_bank) = stream
                                bi = ps_rot.nxt()
                                ps = PS[bi]
                                parts = parts_fn(ci, kk)
                                for pi, (lh, rh) in enumerate(parts):
                                    P.pe(lambda e, ps=ps, lh=lh, rh=rh, pi=pi, n=len(parts): e.matmul(ps[:], lhsT=lh, rhs=rh, start=(pi == 0), stop=(pi == n - 1)),
                                         reads=kids + q_ids, writes=[("ps", bi)])
                                pti = pt_rot.nxt()
                                pt = PT[pti]
                                P.act(lambda e, ps=ps, pt=pt, scale=scale: e.activation(out=pt[:], in_=ps[:], func=AF.Exp, scale=scale), reads=[("ps", bi)], writes=[("PT", pti)])
                                if kt >= 16 * s:
                                    P.dve(lambda e, pt=pt, kt=kt: e.scalar_tensor_tensor(out=pt[:], in0=QPOS[:], scalar=CONST[:, C_KPOS + kt:C_KPOS + kt + 1], in1=pt[:], op0=ALU.is_ge, op1=ALU.mult),
                                          reads=[("PT", pti), "QPOS", "CONST"], writes=[("PT", pti)])
                                return (kt, kk, ci, pti, o_bank, l_bank)

                            def pv(u):
                                kt, kk, ci, pti, o_bank, l_bank = u
                                pt = PT[pti]
                                P.pe(lambda e: e.matmul(PS[o_bank][:], lhsT=VC[ci][:, kk, :], rhs=pt[:], start=(kt == 0), stop=(kt == nkt - 1)),
                                     reads=[("PT", pti), ("VC", ci)], writes=[("ps", o_bank)])
                                P.pe(lambda e: e.matmul(PS[l_bank][:], lhsT=ONES[:], rhs=pt[:], start=(kt == 0), stop=(kt == nkt - 1)),
                                     reads=[("PT", pti), "ONES"], writes=[("ps", l_bank)])

                            prev = None
                            for kt in range(nkt):
                                for stream in streams:
                                    cur = qk(kt, stream)
                                    if prev is not None:
                                        pv(prev)
                                    prev = cur
                            pv(prev)

                        for h in range(8):
                            hb = 64 * (h % 2)
                            attend(kTm, vM, h, True,
                                   [(lambda ci, kk, h=h, hb=hb: [(KC[ci][:, kk * 128:(kk + 1) * 128], QTM[:, h, :]),
                                                                 (KRC[ci][hb:hb + 64, kk * 128:(kk + 1) * 128], QTR[hb:hb + 64, h // 2, :])],
                                     [("QTM", h), ("QTR", h // 2)], 192.0 ** -0.5, 2, 3)])
                            P.dve(lambda e: e.reciprocal(out=RL[:], in_=PS[3][:]), reads=[("ps", 3)], writes=["RL"])
                            P.dve(lambda e, h=h: e.tensor_tensor(out=OTM[:, h, :], in0=PS[2][:], in1=RL[:], op=ALU.mult), reads=[("ps", 2), "RL"], writes=[("OTM", h)])
                        for h in range(8):
                            attend(kTd, vD, h, False,
                                   [(lambda ci, kk, h=h, m=m: [(KC[ci][64 * m:64 * m + 64, kk * 128:(kk + 1) * 128], QTD[64 * m:64 * m + 64, h, :])],
                                     [("QTD", h)], 64.0 ** -0.5, 2 + 2 * m, 3 + 2 * m) for m in range(2)])
                            P.dve(lambda e: e.reciprocal(out=RL[:], in_=PS[3][:]), reads=[("ps", 3)], writes=["RL"])
                            P.dve(lambda e: e.tensor_tensor(out=A1[:], in0=PS[2][:], in1=RL[:], op=ALU.mult), reads=[("ps", 2), "RL"], writes=["A1"])
                            P.dve(lambda e: e.reciprocal(out=RL[:], in_=PS[5][:]), reads=[("ps", 5), "A1"], writes=["RL"])
                            P.dve(lambda e: e.scalar_tensor_tensor(out=A2[:], in0=PS[4][:], scalar=SMALL[:, 0:1], in1=RL[:], op0=ALU.mult, op1=ALU.mult), reads=[("ps", 4), "RL", "NEGLAM"], writes=["A2"])
                            P.pool(lambda e: e.tensor_tensor(out=A1[:], in0=A1[:], in1=A2[:], op=ALU.add), reads=["A1", "A2"], writes=["A1"])
                            P.act(lambda e: e.activation(out=SQD[:, 0, :], in_=A1[:], func=AF.Square), reads=["A1"], writes=["SQD"])
                            rms_from_sq(SQD, 1, ["SQD"], 128, A2, "A2", None, None)
                            P.dve(lambda e, h=h: e.scalar_tensor_tensor(out=OTD[:, h, :], in0=A1[:], scalar=SMALL[:, 1:2], in1=A2[:], op0=ALU.mult, op1=ALU.mult),
                                  reads=["A1", "A2", "GSUBS"], writes=[("OTD", h)])
                        tap("d_otm", OTM[:], [("OTM", h) for h in range(8)])
                        tap("d_otd", OTD[:], [("OTD", h) for h in range(8)])
                        P.barrier()
                with ExitStack() as esC:
                    sbC = lambda n, sh, d: esC.enter_context(nc.sbuf_tensor("%s_%d" % (n, s), sh, d))
                    MG = sbC("MG", [128, 16, T], BF16)
                    M1 = sbC("M1", [128, T], F32)
                    M2 = sbC("M2", [128, T], F32)
                    GT_ = sbC("GT_", [128, T], F32)
                    xgids = [("XG", k) for k in range(16)]
                    rhs_x = lambda k: XG[:, k, :]
                    otm_ids = [("OTM", h) for h in range(8)]
                    otd_ids = [("OTD", h) for h in range(8)]
                    for n in range(16):
                        for br, (wo, OT, oids, mdst, mid) in enumerate(((w_oa, OTM, otm_ids, M1, "M1"), (w_ob, OTD, otd_ids, M2, "M2"))):
                            psg, pgid = linear_chunk(w_gate, 16, (br * 16 + n) * 128, rhs_x, xgids)
                            P.dve(lambda e, psg=psg: e.tensor_tensor(out=GT_[:], in0=psg[:], in1=RSTD[:], op=ALU.mult), reads=[pgid, "RSTD"], writes=["GT_"])
                            P.act(lambda e, br=br, n=n: e.activation(out=GT_[:], in_=GT_[:], func=AF.Sigmoid, bias=CONST[:, C_BG + br * 16 + n:C_BG + br * 16 + n + 1]), reads=["GT_", "CONST"], writes=["GT_"])
                            psy, pyid = linear_chunk(wo, 8, n * 128, lambda k, OT=OT: OT[:, k, :], oids)
                            P.dve(lambda e, psy=psy, mdst=mdst: e.tensor_tensor(out=mdst[:], in0=psy[:], in1=GT_[:], op=ALU.mult), reads=[pyid, "GT_"], writes=[mid])
                        P.pool(lambda e, n=n: e.tensor_tensor(out=MG[:, n, :], in0=M1[:], in1=M2[:], op=ALU.add), reads=["M1", "M2"], writes=[("MG", n)])
                    mg_ids = [("MG", n) for n in range(16)]
                    for n in range(16):
                        ps, pid = linear_chunk(w_out, 16, n * 128, lambda k: MG[:, k, :], mg_ids)
                        P.dve(lambda e, ps=ps, n=n: e.tensor_tensor(out=X32[:, n, :], in0=X32[:, n, :], in1=ps[:], op=ALU.add), reads=[pid, ("X32", n // 4)], writes=[("X32", n // 4)])
                    tap("d_x1", X32[:], [("X32", q) for q in range(4)])
                    P.barrier()

            with ExitStack() as es3:
                sb3 = lambda n, sh, d: es3.enter_context(nc.sbuf_tensor("%s_%d" % (n, s), sh, d))
                QP = SQ
                SKB = sb3("SKB", [128, 2, 128], BF16)
                S32 = sb3("S32", [128, 16, 128], F32)
                TK = sb3("TK", [128, 2, 128], F32)
                V12 = sb3("V12", [128, 2, 16], F32)
                I1 = sb3("I1", [128, 2, 16], mybir.dt.uint32)
                I1F = sb3("I1F", [128, 2, 16], F32)
                CAND = sb3("CAND", [128, 256], F32)
                CAND2 = sb3("CAND2", [128, 256], F32)
                C16 = sb3("C16", [128, 16], F32)
                CI = sb3("CI", [128, 16], mybir.dt.uint32)
                CIH = sb3("CIH", [128, 16], mybir.dt.uint32)
                CIL = sb3("CIL", [128, 16], mybir.dt.uint32)
                CIHF = sb3("CIHF", [128, 16], F32)
                CILF = sb3("CILF", [128, 16], F32)
                OH = sb3("OH", [128, 16, 16], F32)
                SM = sb3("SM", [128, 8], F32)
                EJ = sb3("EJ", [128, 16], F32)
                ROWC = sb3("ROWC", [128, 3, 128], BF16)
                RTT = sb3("RTT", [128, 3, T], F32)
                WT = sb3("WT", [128, 32, T], BF16)
                G8 = [sb3("G8_%d" % i, [128, 8, 128], BF16) for i in range(2)]
                R8 = [sb3("R8_%d" % i, [128, 8, 32], BF16) for i in range(2)]
                VB = [sb3("VB%d" % i, [128, D], BF16) for i in range(6)]
                GA = [sb3("GA%d" % i, [128, T], BF16) for i in range(2)]
                PP = [sb3("PP%d" % i, [128, T], BF16) for i in range(5)]
                g8_rot, vb_rot, ga_rot, pp_rot, ops_rot = Rot(2), Rot(6), Rot(2), Rot(5), Rot(2)

                for q in range(4):
                    P.act(lambda e, q=q: e.activation(out=SQ[:, 4 * q:4 * q + 4, :], in_=X32[:, 4 * q:4 * q + 4, :], func=AF.Square), reads=[("X32", q)], writes=[("SQ", q)])
                rms_from_sq(SQ, 16, [("SQ", q) for q in range(4)], D, RSTD, "RSTD", None, None)
                for k in range(16):
                    P.dve(lambda e, k=k: e.scalar_tensor_tensor(out=XG[:, k, :], in0=X32[:, k, :], scalar=CONST[:, C_G2 + k:C_G2 + k + 1], in1=RSTD[:], op0=ALU.mult, op1=ALU.mult),
                          reads=[("X32", k // 4), "RSTD", "CONST"], writes=[("XG", k)])
                h2ids = [("XG", k) for k in range(16)]
                rhs_h = lambda k: XG[:, k, :]
                si0 = wst_rot.nxt()
                P.dma(WST[si0][:, 0:2, :], skT.rearrange("c p n -> p c n"), writes=[("WST", si0)])
                P.pool(lambda e: e.tensor_copy(out=SKB[:], in_=WST[si0][:, 0:2, :]), reads=[("WST", si0)], writes=["SKB"])
                for n in range(16):
                    ps, pid = linear_chunk(w_qp, 16, n * 128, rhs_h, h2ids)
                    P.act(lambda e, ps=ps, n=n: e.activation(out=QP[:, n, :], in_=ps[:], func=AF.Copy), reads=[pid], writes=[("SQ", n // 4)])

                for j4 in range(4):
                    tsl = slice(j4 * 128, (j4 + 1) * 128)
                    for g in range(4):
                        bi = 3 + (g % 2)
                        for jj in range(4):
                            n = g * 4 + jj
                            P.pe(lambda e, bi=bi, jj=jj, n=n, tsl=tsl: e.matmul(PS[bi][:, jj * 128:(jj + 1) * 128], lhsT=QP[:, n, tsl], rhs=SKB[:, n % 2, :], start=True, stop=True),
                                 reads=[("SQ", n // 4), "SKB"], writes=[("ps", bi)])
                        P.act(lambda e, bi=bi, g=g: e.activation(out=S32[:, 4 * g:4 * g + 4, :], in_=PS[bi][:].rearrange("p (a b) -> p a b", a=4), func=AF.Copy), reads=[("ps", bi)], writes=[("S32", g)])
                    for h in range(8):
                        sid = ("S32", h // 2)
                        for half in range(2):
                            sc = S32[:, 2 * h + half, :]
                            tk = TK[:, half, :]
                            P.dve(lambda e, sc=sc, half=half: e.max(out=V12[:, half, 0:8], in_=sc), reads=[sid], writes=["V12"])
                            P.dve(lambda e, sc=sc, half=half: e.max_index(out=I1[:, half, 0:8], in_max=V12[:, half, 0:8], in_values=sc), reads=[sid, "V12"], writes=["I1"])
                            P.dve(lambda e, sc=sc, tk=tk, half=half: e.match_replace(out=tk, in_to_replace=V12[:, half, 0:8], in_values=sc, imm_value=-1e30), reads=[sid, "V12"], writes=["TK"])
                            P.dve(lambda e, tk=tk, half=half: e.max(out=V12[:, half, 8:16], in_=tk), reads=["TK"], writes=["V12"])
                            P.dve(lambda e, tk=tk, half=half: e.max_index(out=I1[:, half, 8:16], in_max=V12[:, half, 8:16], in_values=tk), reads=["TK", "V12"], writes=["I1"])
                        P.dve(lambda e: e.tensor_copy(out=I1F[:], in_=I1[:]), reads=["I1"], writes=["I1F"])
                        P.dve(lambda e: e.tensor_tensor(out=CAND[:].rearrange("p (a b) -> p a b", a=16), in0=V12[:, 0, :].unsqueeze(2).broadcast_to([128, 16, 16]),
                                                        in1=V12[:, 1, :].unsqueeze(1).broadcast_to([128, 16, 16]), op=ALU.add), reads=["V12"], writes=["CAND"])
                        P.dve(lambda e: e.max(out=C16[:, 0:8], in_=CAND[:]), reads=["CAND"], writes=["C16"])
                        P.dve(lambda e: e.max_index(out=CI[:, 0:8], in_max=C16[:, 0:8], in_values=CAND[:]), reads=["CAND", "C16"], writes=["CI"])
                        P.dve(lambda e: e.match_replace(out=CAND2[:], in_to_replace=C16[:, 0:8], in_values=CAND[:], imm_value=-1e30), reads=["CAND", "C16"], writes=["CAND2"])
                        P.dve(lambda e: e.max(out=C16[:, 8:16], in_=CAND2[:]), reads=["CAND2"], writes=["C16"])
                        P.dve(lambda e: e.max_index(out=CI[:, 8:16], in_max=C16[:, 8:16], in_values=CAND2[:]), reads=["CAND2", "C16"], writes=["CI"])
                        P.dve(lambda e: e.tensor_scalar(out=SM[:, 0:1], in0=C16[:, 0:1], scalar1=-1.0, scalar2=None, op0=ALU.mult), reads=["C16"], writes=["SM0"])
                        P.act(lambda e: e.activation(out=EJ[:], in_=C16[:], func=AF.Exp, bias=SM[:, 0:1], accum_out=SM[:, 1:2]), reads=["C16", "SM0"], writes=["EJ", "SM1"])
                        P.dve(lambda e: e.reciprocal(out=SM[:, 2:3], in_=SM[:, 1:2]), reads=["SM1"], writes=["SM2"])
                        P.dve(lambda e, h=h: e.tensor_scalar(out=ROWC[:, 2, h * 16:(h + 1) * 16], in0=EJ[:], scalar1=SM[:, 2:3], scalar2=None, op0=ALU.mult), reads=["EJ", "SM2"], writes=[("ROWC", 2, h)])
                        P.dve(lambda e: e.tensor_single_scalar(out=CIH[:], in_=CI[:], scalar=4, op=ALU.logical_shift_right), reads=["CI"], writes=["CIH"])
                        P.dve(lambda e: e.tensor_single_scalar(out=CIL[:], in_=CI[:], scalar=15, op=ALU.bitwise_and), reads=["CI"], writes=["CIL"])
                        P.dve(lambda e: e.tensor_copy(out=CIHF[:], in_=CIH[:]), reads=["CIH"], writes=["CIHF"])
                        P.dve(lambda e: e.tensor_copy(out=CILF[:], in_=CIL[:]), reads=["CIL"], writes=["CILF"])
                        for which, (cf, cid) in enumerate(((CIHF, "CIHF"), (CILF, "CILF"))):
                            P.dve(lambda e, cf=cf: e.tensor_tensor(out=OH[:], in0=CONST[:, C_IOTA:C_IOTA + 16].unsqueeze(1).broadcast_to([128, 16, 16]),
                                                                   in1=cf[:].unsqueeze(2).broadcast_to([128, 16, 16]), op=ALU.is_equal), reads=["CONST", cid, "EJ"], writes=["OH"])
                            P.dve(lambda e, which=which: e.tensor_tensor(out=OH[:], in0=OH[:], in1=I1F[:, which, :].unsqueeze(1).broadcast_to([128, 16, 16]), op=ALU.mult), reads=["OH", "I1F"], writes=["OH"])
                            P.dve(lambda e: e.reduce_sum(out=EJ[:], in_=OH[:], axis=mybir.AxisListType.X), reads=["OH"], writes=["EJ"])
                            P.dve(lambda e, which=which, h=h: e.tensor_copy(out=ROWC[:, which, h * 16:(h + 1) * 16], in_=EJ[:]), reads=["EJ"], writes=[("ROWC", which, h)])
                    rc_ids = [("ROWC", w, h) for w in range(3) for h in range(8)]
                    for w in range(3):
                        P.pe(lambda e, w=w: e.matmul(PS[5][:, w * 128:(w + 1) * 128], lhsT=ROWC[:, w, :], rhs=IDB[:], start=True, stop=True), reads=rc_ids + ["IDB"], writes=[("ps", 5)])
                    P.act(lambda e, tsl=tsl: e.activation(out=RTT[:, :, tsl], in_=PS[5][:, 0:384].rearrange("p (a b) -> p a b", a=3), func=AF.Copy), reads=[("ps", 5)], writes=[("RTT", j4)])
                rtt_ids = [("RTT", j) for j in range(4)]
                tap("d_rtt", RTT[:], rtt_ids)
                tap("d_h2", XG[:], h2ids)
                tap("d_qp", QP[:], [("SQ", q) for q in range(4)])

                for qr in range(4):
                    for b8 in range(T // 8):
                        gi = g8_rot.nxt()
                        ts8 = slice(b8 * 8, b8 * 8 + 8)
                        P.dve(lambda e, gi=gi, ts8=ts8: e.tensor_tensor(out=G8[gi][:], in0=CONST[:, C_IOTA:C_IOTA + 128].unsqueeze(1).broadcast_to([128, 8, 128]),
                                                                       in1=RTT[:, 1, ts8].unsqueeze(2).broadcast_to([128, 8, 128]), op=ALU.is_equal), reads=rtt_ids + ["CONST"], writes=[("G8", gi)])
                        P.dve(lambda e, gi=gi, ts8=ts8, qr=qr: e.tensor_tensor(out=R8[gi][:], in0=CONST[:, C_IOTA + 32 * qr:C_IOTA + 32 * qr + 32].unsqueeze(1).broadcast_to([128, 8, 32]),
                                                                              in1=RTT[:, 0, ts8].unsqueeze(2).broadcast_to([128, 8, 32]), op=ALU.is_equal), reads=rtt_ids + ["CONST"], writes=[("R8", gi)])
                        P.pool(lambda e, gi=gi, ts8=ts8: e.tensor_tensor(out=R8[gi][:], in0=R8[gi][:], in1=RTT[:, 2, ts8].unsqueeze(2).broadcast_to([128, 8, 32]), op=ALU.mult), reads=rtt_ids + [("R8", gi)], writes=[("R8", gi)])
                        for j in range(8):
                            P.pe(lambda e, gi=gi, j=j: e.matmul(PS[5][:, j * 32:(j + 1) * 32], lhsT=G8[gi][:, j, :], rhs=R8[gi][:, j, :], start=True, stop=True),
                                 reads=[("G8", gi), ("R8", gi)], writes=[("ps", 5)])
                        P.act(lambda e, ts8=ts8: e.activation(out=WT[:, :, ts8], in_=PS[5][:, 0:256].rearrange("p (t r) -> p r t", t=8), func=AF.Copy), reads=[("ps", 5)], writes=["WT"])
                    if qr == 0:
                        tap("d_wt", WT[:], ["WT"])
                    for g in range(8):
                        grp = []
                        for jj in range(4):
                            rl = g * 4 + jj
                            r = qr * 32 + rl
                            ub, ubid = stream_w(ut, 16, r * 128)
                            si, vi = wst_rot.nxt(), vb_rot.nxt()
                            P.dma(WST[si][:].rearrange("p k n -> p (k n)"), ev[r * 128:(r + 1) * 128, :], writes=[("WST", si)])
                            (P.dve if (rl % 2 == 0) else P.pool)(lambda e, si=si, vi=vi: e.tensor_copy(out=VB[vi][:], in_=WST[si][:].rearrange("p k n -> p (k n)")), reads=[("WST", si)], writes=[("VB", vi)])
                            bi = ps_rot.nxt()
                            for k in range(16):
                                P.pe(lambda e, bi=bi, ub=ub, k=k: e.matmul(PS[bi][:], lhsT=ub[:, k, :], rhs=XG[:, k, :], start=(k == 0), stop=(k == 15)),
                                     reads=[ubid] + h2ids, writes=[("ps", bi)])
                            gi, pi = ga_rot.nxt(), pp_rot.nxt()
                            P.act(lambda e, bi=bi, gi=gi: e.activation(out=GA[gi][:], in_=PS[bi][:], func=AF.Gelu), reads=[("ps", bi)], writes=[("GA", gi)])
                            P.dve(lambda e, gi=gi, pi=pi, rl=rl: e.tensor_tensor(out=PP[pi][:], in0=GA[gi][:], in1=WT[:, rl, :], op=ALU.mult), reads=[("GA", gi), "WT"], writes=[("PP", pi)])
                            grp.append((vi, pi))
                        for n in range(16):
                            bi = 6 + ops_rot.nxt()
                            for jj, (vi, pi) in enumerate(grp):
                                P.pe(lambda e, bi=bi, vi=vi, pi=pi, jj=jj, n=n: e.matmul(PS[bi][:], lhsT=VB[vi][:, n * 128:(n + 1) * 128], rhs=PP[pi][:], start=(jj == 0), stop=(jj == 3)),
                                     reads=[("VB", vi), ("PP", pi)], writes=[("ps", bi)])
                            P.dve(lambda e, bi=bi, n=n: e.tensor_tensor(out=X32[:, n, :], in0=X32[:, n, :], in1=PS[bi][:], op=ALU.add), reads=[("ps", bi), ("X32", n // 4)], writes=[("X32", n // 4)])

                tap("d_xpre", X32[:], [("X32", q) for q in range(4)])
                for q in range(4):
                    P.act(lambda e, q=q: e.activation(out=SQ[:, 4 * q:4 * q + 4, :], in_=X32[:, 4 * q:4 * q + 4, :], func=AF.Square), reads=[("X32", q)], writes=[("SQ", q)])
                rms_from_sq(SQ, 16, [("SQ", q) for q in range(4)], D, RSTD, "RSTD", None, None)
                for k in range(16):
                    P.dve(lambda e, k=k: e.scalar_tensor_tensor(out=X32[:, k, :], in0=X32[:, k, :], scalar=CONST[:, C_GF + k:C_GF + k + 1], in1=RSTD[:], op0=ALU.mult, op1=ALU.mult),
                          reads=[("X32", k // 4), "RSTD", "CONST"], writes=[("X32", k // 4)])
                for q in range(4):
                    st = P.dma(outT[q * 512:(q + 1) * 512, t0:t0 + T].rearrange("(k p) t -> p k t", p=128), X32[:, 4 * q:4 * q + 4, :],
                               reads=[("X32", q)], writes=[("out", s, q)], semkey=("ost", q))
                    final_stores.append(st)
                P.barrier()

    for s_ in range(NSLOT):
        run_slot(s_)
    P.finish(final_waits=final_stores)
    es.close()
    return nc


OFF_CQ, OFF_CKV, OFF_KR, OFF_DQ, OFF_DK, OFF_DV, OFF_GATE = 0, 768, 1280, 1344, 2368, 3392, 4416


def _perm64(cols):
    cols = np.asarray(cols).reshape(-1, 64)
    return np.concatenate([cols[:, 32:], cols[:, :32]], axis=1).reshape(-1)


def _rope_tables(pos):
    inv = (10000.0 ** (-np.arange(0, 64, 2, dtype=np.float32) / np.float32(64))).astype(np.float32)
    ang = (pos.astype(np.float32)[None, :] * inv[:, None]).astype(np.float32)
    cos = np.cos(ang.astype(np.float64)).astype(np.float32)
    sin = np.sin(ang.astype(np.float64)).astype(np.float32)
    cos64 = np.concatenate([cos, cos], 0)
    sin64 = np.concatenate([-sin, sin], 0)
    return np.ascontiguousarray(np.stack([np.concatenate([cos64, cos64], 0), np.concatenate([sin64, sin64], 0)], 0))


_NC_CACHE = {}


def kernel(x, w_in, b_gate, g_norm1, g_cq, w_uq, g_ckv, w_ukv, w_o_mla, lambda_qk, g_subln, w_o_diff,
           w_out, g_norm2, w_q_peer, sub_keys, expert_u, expert_v, g_final):
    f = lambda a: np.asarray(a, dtype=np.float32)
    x = f(x)
    w_in0 = f(w_in)[0]
    ar = np.arange
    ckv = ar(OFF_CKV, OFF_KR)
    kr = ar(OFF_KR, OFF_DQ)
    kv_cols = [ckv, kr, kr, _perm64(kr), _perm64(kr)]
    for h in range(8):
        dk = ar(OFF_DK + h * 128, OFF_DK + (h + 1) * 128)
        kv_cols += [dk, _perm64(dk)]
    W_kv = np.ascontiguousarray(w_in0[:, np.concatenate(kv_cols)])
    W_dv = np.ascontiguousarray(w_in0[:, OFF_DV:OFF_GATE])
    q_cols = [ar(OFF_CQ, OFF_CKV)]
    for h in range(8):
        dq = ar(OFF_DQ + h * 128, OFF_DQ + (h + 1) * 128)
        q_cols += [dq, _perm64(dq)]
    W_q = np.ascontiguousarray(w_in0[:, np.concatenate(q_cols)])
    W_gate = np.ascontiguousarray(w_in0[:, OFF_GATE:])
    wuq = f(w_uq)[0]
    nope = np.concatenate([ar(h * 192, h * 192 + 128) for h in range(8)])
    rope = np.concatenate([ar(h * 192 + 128, h * 192 + 192) for h in range(8)])
    W_uq2 = np.ascontiguousarray(wuq[:, np.concatenate([nope, rope, _perm64(rope)])])
    wukv = f(w_ukv)[0]
    W_uk = np.ascontiguousarray(wukv[:, np.concatenate([ar(h * 256, h * 256 + 128) for h in range(8)])])
    W_uv = np.ascontiguousarray(wukv[:, np.concatenate([ar(h * 256 + 128, h * 256 + 256) for h in range(8)])])
    skT = np.ascontiguousarray(f(sub_keys)[0].transpose(0, 2, 1))
    ut = np.ascontiguousarray(f(expert_u)[0].T)
    ev = np.ascontiguousarray(f(expert_v)[0])
    col = lambda v: np.ascontiguousarray(f(v).reshape(-1, 128).T)
    consts = np.zeros((128, NC_CONST), np.float32)
    consts[:, C_G1:C_G1 + 16] = col(g_norm1[0])
    consts[:, C_GCQ:C_GCQ + 6] = col(g_cq[0])
    consts[:, C_GCKV:C_GCKV + 4] = col(g_ckv[0])
    consts[:, C_BG:C_BG + 32] = col(b_gate[0])
    consts[:, C_GSUB:C_GSUB + 1] = col(g_subln[0])
    consts[:, C_G2:C_G2 + 16] = col(g_norm2[0])
    consts[:, C_GF:C_GF + 16] = col(g_final)
    consts[:, C_LAM:C_LAM + 256] = f(lambda_qk)[0].reshape(1, 256)
    consts[:, C_KPOS:C_KPOS + 64] = (ar(64)[None, :] * 128 + ar(128)[:, None]).astype(np.float32)
    consts[:, C_IOTA:C_IOTA + 128] = ar(128, dtype=np.float32)[None, :]
    consts[:, C_IDENT:C_IDENT + 128] = np.eye(128, dtype=np.float32)
    cs_seq = _rope_tables(ar(SEQ))
    shared = {"consts": consts, "cs_seq": cs_seq, "w_kv": W_kv, "w_dv": W_dv, "w_q": W_q, "w_gate": W_gate,
              "w_uq2": W_uq2, "w_uk": W_uk, "w_uv": W_uv, "w_oa": np.ascontiguousarray(f(w_o_mla)[0]),
              "w_ob": np.ascontiguousarray(f(w_o_diff)[0]), "w_out": np.ascontiguousarray(f(w_out)[0]),
              "w_qp": np.ascontiguousarray(f(w_q_peer)[0]), "skT": skT, "ut": ut, "ev": ev}
    in_maps, own_pos = [], []
    xT = [np.ascontiguousarray(x[b].T) for b in range(2)]
    for c in range(8):
        b, p = c // 4, c % 4
        groups = [p, 7 - p, 8 + p, 15 - p]
        pos = np.concatenate([ar(g * 512, (g + 1) * 512) for g in groups])
        own_pos.append((b, pos))
        m = dict(shared)
        m["xT_seq"] = xT[b]
        m["xT_own"] = np.ascontiguousarray(xT[b][:, pos])
        m["cs_own"] = _rope_tables(pos)
        m["qpos"] = np.ascontiguousarray(np.broadcast_to(pos.astype(np.float32)[None, :], (128, 2048)))
        in_maps.append(m)
    if "nc" not in _NC_CACHE:
        _NC_CACHE["nc"] = build_program()
    res = run_bass_kernel_spmd(_NC_CACHE["nc"], in_maps, core_ids=list(range(8)))
    if DEBUG:
        _DBG["res"] = res.results
    out = np.zeros((2, SEQ, D), np.float32)
    for c in range(8):
        b, pos = own_pos[c]
        out[b, pos, :] = res.results[c]["outT"].T
    return out
```
